# Optimizing a Trainium2 kernel written in Bass

```python
import jax
import jax.numpy as jnp
from jax import lax
import numpy as np

D_MODEL = 1024
BATCH = 2
SEQ = 8192
DEPTH = 1

EPS = 1e-6
N_MOD = 6

GLA_HEADS = 4
GLA_DK = 64
GLA_DV = 128
GLA_KW = GLA_HEADS * GLA_DK
GLA_WIDTH = GLA_HEADS * GLA_DV
GLA_GATE_RANK = 16
GLA_GATE_NORM = 16.0
GLA_CHUNK = 16

SWA_HEADS = 8
SWA_KV_HEADS = 2
SWA_HD = 64
SWA_WIDTH = SWA_HEADS * SWA_HD
SWA_KVW = SWA_KV_HEADS * SWA_HD
WINDOW = 128
ROPE_THETA = 500000.0
ROPE_DIM = SWA_HD // 4

MIX_WIDTH = GLA_WIDTH + SWA_WIDTH
IN_SPLITS = (GLA_KW, GLA_KW, GLA_WIDTH, GLA_GATE_RANK, GLA_WIDTH, SWA_WIDTH, SWA_KVW, SWA_KVW)
IN_WIDTH = sum(IN_SPLITS)

N_EXPERTS = 256
TOP_K = 8
N_GROUPS = 8
TOPK_GROUPS = 4
EXPERT_FF = 256
SHARED_FF = 256
ROUTED_SCALE = 2.5
MOE_BLOCK = 128

kernel_name = 'hybrid_gla_swa_sink_moe_adaln_block'


def rmsnorm(x, g):
    xf = x.astype(jnp.float32)
    y = xf * lax.rsqrt(jnp.mean(xf * xf, axis=-1, keepdims=True) + EPS)
    return (y * g.astype(jnp.float32)).astype(x.dtype)


def split_cols(t, sizes):
    out, start = [], 0
    for s in sizes:
        out.append(t[..., start:start + s])
        start += s
    return out


def partial_rope(t, cos, sin):
    half = ROPE_DIM // 2
    t1 = t[..., :half]
    t2 = t[..., half:ROPE_DIM]
    cos = cos.astype(t.dtype)
    sin = sin.astype(t.dtype)
    return jnp.concatenate([t1 * cos - t2 * sin, t2 * cos + t1 * sin, t[..., ROPE_DIM:]], axis=-1)


def gla_heads(q, k, v, log_a):
    B, S = q.shape[0], q.shape[1]
    nc = S // GLA_CHUNK

    def chunks(t):
        return t.reshape(B, nc, GLA_CHUNK, GLA_HEADS, -1).transpose(0, 3, 1, 2, 4).astype(jnp.float32)

    qc = chunks(q) * (GLA_DK ** -0.5)
    kc = chunks(k)
    vc = chunks(v)
    b = jnp.cumsum(chunks(log_a), axis=3)
    b_last = b[:, :, :, -1:, :]

    causal = jnp.tril(jnp.ones((GLA_CHUNK, GLA_CHUNK), dtype=bool))
    diff = b[..., :, None, :] - b[..., None, :, :]
    decay = jnp.exp(jnp.where(causal[:, :, None], diff, -jnp.inf))
    attn = jnp.einsum('bhnid,bhnjd,bhnijd->bhnij', qc, kc, decay)
    o_intra = jnp.einsum('bhnij,bhnjv->bhniv', attn, vc)

    q_dec = jnp.moveaxis(qc * jnp.exp(b), 2, 0)
    k_dec = jnp.moveaxis(kc * jnp.exp(b_last - b), 2, 0)
    v_s = jnp.moveaxis(vc, 2, 0)
    chunk_decay = jnp.moveaxis(jnp.exp(b_last[:, :, :, 0, :]), 2, 0)

    def step(state, inp):
        qd, kd, vv, dec = inp
        o = jnp.einsum('bhid,bhdv->bhiv', qd, state)
        new_state = dec[..., None] * state + jnp.einsum('bhid,bhiv->bhdv', kd, vv)
        return new_state, o

    s0 = jnp.zeros((B, GLA_HEADS, GLA_DK, GLA_DV), jnp.float32)
    _, o_inter = lax.scan(step, s0, (q_dec, k_dec, v_s, chunk_decay))
    o = o_intra + jnp.moveaxis(o_inter, 0, 2)
    return o.transpose(0, 2, 3, 1, 4).reshape(B, S, GLA_HEADS, GLA_DV)


def swa_sink_heads(q, k, v, sinks):
    B, S = q.shape[0], q.shape[1]
    nb = S // WINDOW
    G = SWA_HEADS // SWA_KV_HEADS
    qb = q.reshape(B, nb, WINDOW, SWA_KV_HEADS, G, SWA_HD)

    def band(t):
        tp = jnp.pad(t, ((0, 0), (WINDOW, 0), (0, 0), (0, 0))).reshape(B, nb + 1, WINDOW, SWA_KV_HEADS, SWA_HD)
        return jnp.concatenate([tp[:, :-1], tp[:, 1:]], axis=2)

    kb = band(k)
    vb = band(v)
    s = jnp.einsum('bnqkgd,bnskd->bnkgqs', qb, kb).astype(jnp.float32) * (SWA_HD ** -0.5)
    qi = jnp.arange(WINDOW)[:, None]
    kj = jnp.arange(2 * WINDOW)[None, :]
    rel = qi + WINDOW - kj
    in_window = (rel >= 0) & (rel < WINDOW)
    key_exists = (jnp.arange(nb)[:, None, None] * WINDOW - WINDOW + kj[None]) >= 0
    mask = in_window[None] & key_exists
    s = jnp.where(mask[None, :, None, None], s, -jnp.inf)
    sink = sinks.astype(jnp.float32).reshape(SWA_KV_HEADS, G)[None, None, :, :, None, None]
    m = jnp.maximum(jnp.max(s, axis=-1, keepdims=True), sink)
    p = jnp.exp(s - m)
    p = p / (jnp.sum(p, axis=-1, keepdims=True) + jnp.exp(sink - m))
    o = jnp.einsum('bnkgqs,bnskd->bnqkgd', p.astype(v.dtype), vb)
    return o.reshape(B, S, SWA_WIDTH)


def route(xf, w_router, router_bias):
    N = xf.shape[0]
    scores = jax.nn.sigmoid(xf.astype(jnp.float32) @ w_router.astype(jnp.float32))
    sel = scores + router_bias.astype(jnp.float32)
    grp = sel.reshape(N, N_GROUPS, N_EXPERTS // N_GROUPS)
    grp_score = jnp.sum(lax.top_k(grp, 2)[0], axis=-1)
    _, gidx = lax.top_k(grp_score, TOPK_GROUPS)
    gmask = jnp.any(gidx[..., None] == jnp.arange(N_GROUPS), axis=1)
    sel = jnp.where(jnp.repeat(gmask, N_EXPERTS // N_GROUPS, axis=1), sel, -jnp.inf)
    _, idx = lax.top_k(sel, TOP_K)
    w = jnp.take_along_axis(scores, idx, axis=-1)
    w = w / jnp.sum(w, axis=-1, keepdims=True) * ROUTED_SCALE
    return idx, w


def routed_experts(xf, idx, w, w_gate, w_up, w_down):
    N, D = xf.shape
    A = N * TOP_K
    flat_e = idx.reshape(-1)
    order = jnp.argsort(flat_e)
    sorted_e = flat_e[order]
    tok = order // TOP_K
    counts = jnp.zeros((N_EXPERTS,), jnp.int32).at[flat_e].add(1)
    padded = (counts + MOE_BLOCK - 1) // MOE_BLOCK * MOE_BLOCK
    pad_end = jnp.cumsum(padded)
    pad_start = pad_end - padded
    start = jnp.cumsum(counts) - counts
    dest = pad_start[sorted_e] + jnp.arange(A) - start[sorted_e]
    n_blocks = (A + N_EXPERTS * (MOE_BLOCK - 1) + MOE_BLOCK - 1) // MOE_BLOCK
    rows = n_blocks * MOE_BLOCK
    xs = jnp.zeros((rows, D), xf.dtype).at[dest].set(xf[tok])
    block_e = jnp.minimum(jnp.searchsorted(pad_end, jnp.arange(n_blocks) * MOE_BLOCK, side='right'), N_EXPERTS - 1)

    def expert_block(args):
        xb, e = args
        h = jax.nn.silu(xb @ w_gate[e]) * (xb @ w_up[e])
        return h @ w_down[e]

    ys = lax.map(expert_block, (xs.reshape(n_blocks, MOE_BLOCK, D), block_e)).reshape(rows, D)
    contrib = ys[dest] * w.reshape(-1)[order][:, None].astype(ys.dtype)
    return jnp.zeros_like(xf).at[tok].add(contrib)


def setup_inputs(seed: int = 0) -> dict:
    key = jax.random.key(seed)
    ks = jax.random.split(key, 24)
    f32 = jnp.float32
    L, D = DEPTH, D_MODEL

    def nrm(k, shape, scale):
        return jax.random.normal(k, shape, f32) * scale

    x = jax.random.normal(ks[0], (BATCH, SEQ, D), f32)
    c = jax.random.normal(ks[1], (BATCH, D), f32)
    positions = jnp.broadcast_to(jnp.arange(SEQ, dtype=jnp.int32), (BATCH, SEQ))
    return {
        'x': x,
        'c': c,
        'positions': positions,
        'w_ada': nrm(ks[2], (L, D, N_MOD * D), 0.5 * D ** -0.5),
        'b_ada': nrm(ks[3], (L, N_MOD * D), 0.02),
        'g_attn': 1.0 + nrm(ks[4], (L, D), 0.02),
        'w_in': nrm(ks[5], (L, D, IN_WIDTH), D ** -0.5),
        'w_gk_up': nrm(ks[6], (L, GLA_GATE_RANK, GLA_KW), GLA_GATE_RANK ** -0.5),
        'b_gk': nrm(ks[7], (L, GLA_KW), 0.1),
        'g_gla_out': 1.0 + nrm(ks[8], (L, GLA_DV), 0.02),
        'sinks': nrm(ks[9], (L, SWA_HEADS), 0.5),
        'w_out': nrm(ks[10], (L, MIX_WIDTH, D), MIX_WIDTH ** -0.5),
        'g_ffn': 1.0 + nrm(ks[11], (L, D), 0.02),
        'w_router': nrm(ks[12], (L, D, N_EXPERTS), D ** -0.5),
        'router_bias': nrm(ks[13], (L, N_EXPERTS), 0.01),
        'w_gate': nrm(ks[14], (L, N_EXPERTS, D, EXPERT_FF), D ** -0.5),
        'w_up': nrm(ks[15], (L, N_EXPERTS, D, EXPERT_FF), D ** -0.5),
        'w_down': nrm(ks[16], (L, N_EXPERTS, EXPERT_FF, D), EXPERT_FF ** -0.5),
        'ws_gate': nrm(ks[17], (L, D, SHARED_FF), D ** -0.5),
        'ws_up': nrm(ks[18], (L, D, SHARED_FF), D ** -0.5),
        'ws_down': nrm(ks[19], (L, SHARED_FF, D), SHARED_FF ** -0.5),
        'g_final': 1.0 + nrm(ks[20], (D,), 0.02),
    }


def reference(x, c, positions, w_ada, b_ada, g_attn, w_in, w_gk_up, b_gk, g_gla_out, sinks, w_out,
              g_ffn, w_router, router_bias, w_gate, w_up, w_down, ws_gate, ws_up, ws_down, g_final):
    B, S, D = x.shape
    inv_freq = jnp.power(ROPE_THETA, -jnp.arange(0, ROPE_DIM, 2, dtype=jnp.float32) / ROPE_DIM)
    ang = positions.astype(jnp.float32)[..., None] * inv_freq
    cos = jnp.cos(ang)[:, :, None, :]
    sin = jnp.sin(ang)[:, :, None, :]
    c_act = jax.nn.silu(c)

    for l in range(DEPTH):
        mod = c_act @ w_ada[l] + b_ada[l]
        sh1, sc1, gt1, sh2, sc2, gt2 = jnp.split(mod, N_MOD, axis=-1)

        h = rmsnorm(x, g_attn[l]) * (1.0 + sc1[:, None]) + sh1[:, None]
        proj = h @ w_in[l]
        gq, gk, gv, g_low, g_r, sq, sk, sv = split_cols(proj, IN_SPLITS)

        log_a = jax.nn.log_sigmoid((g_low @ w_gk_up[l] + b_gk[l]).astype(jnp.float32)) / GLA_GATE_NORM
        o_gla = gla_heads(gq.reshape(B, S, GLA_HEADS, GLA_DK), gk.reshape(B, S, GLA_HEADS, GLA_DK),
                          gv.reshape(B, S, GLA_HEADS, GLA_DV), log_a.reshape(B, S, GLA_HEADS, GLA_DK))
        o_gla = rmsnorm(o_gla, g_gla_out[l]).astype(x.dtype)
        o_gla = (o_gla * jax.nn.silu(g_r.reshape(B, S, GLA_HEADS, GLA_DV))).reshape(B, S, GLA_WIDTH)

        q = partial_rope(sq.reshape(B, S, SWA_HEADS, SWA_HD), cos, sin)
        k = partial_rope(sk.reshape(B, S, SWA_KV_HEADS, SWA_HD), cos, sin)
        v = sv.reshape(B, S, SWA_KV_HEADS, SWA_HD)
        o_swa = swa_sink_heads(q, k, v, sinks[l])

        mixed = jnp.concatenate([o_gla, o_swa.astype(x.dtype)], axis=-1) @ w_out[l]
        x = x + gt1[:, None] * mixed

        h = rmsnorm(x, g_ffn[l]) * (1.0 + sc2[:, None]) + sh2[:, None]
        hf = h.reshape(B * S, D)
        shared = (jax.nn.silu(hf @ ws_gate[l]) * (hf @ ws_up[l])) @ ws_down[l]
        idx, wts = route(hf, w_router[l], router_bias[l])
        routed = routed_experts(hf, idx, wts, w_gate[l], w_up[l], w_down[l])
        x = x + gt2[:, None] * (shared + routed).reshape(B, S, D)

    return rmsnorm(x, g_final)
```

```python
import math
import os
from contextlib import ExitStack

import numpy as np
import concourse.bass as bass
import concourse.mybir as mybir
from concourse.bass_utils import run_bass_kernel_spmd

F32 = mybir.dt.float32
BF16 = mybir.dt.bfloat16
I32 = mybir.dt.int32
AF = mybir.ActivationFunctionType
ALU = mybir.AluOpType
AX = mybir.AxisListType

ENGS = ['pe', 'act', 'dve', 'pool', 'sp']
NDMA = 24

N_CORES = 8
D = 1024
KC = 8
T = 128
NT = 16
NPRE = 48
TOK = NT * T
NEXP = 256
EPS = 1e-6
NEG = -30000.0

C_GQ, C_GK, C_GV, C_GLOW, C_GR, C_SQ, C_SK, C_SV = 0, 256, 512, 1024, 1040, 1552, 2064, 2192
IN_W = 2320


class Res:
    __slots__ = ('name', 'w', 'r', 'excl')

    def __init__(self, name, excl=False):
        self.name = name
        self.w = None
        self.r = {}
        self.excl = excl


class Sched:
    def __init__(self, nc, es):
        self.nc = nc
        self.ops = {e: [] for e in ENGS}
        self.esem = {e: es.enter_context(nc.semaphore('s_' + e)) for e in ENGS}
        self.dsem = [es.enter_context(nc.semaphore('d%d' % i)) for i in range(NDMA)]
        self.dcount = [0] * NDMA
        self.dnext = 0
        self.waited = {e: {} for e in ENGS}

    def _need(self, eng, ev, waits, is_dma):
        if ev is None:
            return
        if ev[0] == 'e':
            _, E, i = ev
            if E == eng and eng == 'pe' and not is_dma:
                return
            if E == eng and eng == 'sp':
                return
            key = ('e', E)
            if self.waited[eng].get(key, -1) >= i:
                return
            self.waited[eng][key] = i
            self.ops[E][i]['signal'] = True
            waits.append(ev)
        else:
            _, k, v = ev
            key = ('d', k)
            if self.waited[eng].get(key, 0) >= v:
                return
            self.waited[eng][key] = v
            waits.append(ev)

    def op(self, eng, fn, reads=(), writes=(), dma=False):
        waits = []
        for R in reads:
            self._need(eng, R.w, waits, dma)
            if R.excl:
                for ev in R.r.values():
                    if ev[0] == 'e' and ev[1] == eng:
                        continue
                    self._need(eng, ev, waits, dma)
        for R in writes:
            self._need(eng, R.w, waits, dma)
            for ev in R.r.values():
                self._need(eng, ev, waits, dma)
        rec = {'fn': fn, 'waits': waits, 'signal': False, 'dma': None}
        idx = len(self.ops[eng])
        if dma:
            k = self.dnext
            self.dnext = (self.dnext + 1) % NDMA
            if self.dcount[k] > 0:
                self._need(eng, ('d', k, self.dcount[k]), waits, dma)
            self.dcount[k] += 16
            rec['dma'] = k
            ev = ('d', k, self.dcount[k])
        else:
            ev = ('e', eng, idx)
        self.ops[eng].append(rec)
        for R in reads:
            R.r[ev[:2]] = ev
        for R in writes:
            R.w = ev
            R.r = {}
        return ev

    def wait_all(self, eng):
        waits = []
        for E in ENGS:
            if E != eng and E != 'sp' and self.ops[E]:
                i = len(self.ops[E]) - 1
                while i >= 0 and (self.ops[E][i]['fn'] is None or self.ops[E][i]['dma'] is not None):
                    i -= 1
                if i >= 0:
                    self._need(eng, ('e', E, i), waits, False)
        for k in range(NDMA):
            if self.dcount[k] > 0:
                self._need(eng, ('d', k, self.dcount[k]), waits, False)
        self.ops[eng].append({'fn': None, 'waits': waits, 'signal': False, 'dma': None})

    def barrier(self):
        for E in ENGS:
            self.wait_all(E)

    def emit(self):
        nc = self.nc
        pref = {}
        for E in ENGS:
            c = 0
            arr = []
            for o in self.ops[E]:
                if o['signal']:
                    c += 1
                arr.append(c)
            pref[E] = arr
        stats = {}

        def run(E, eng):
            nw = 0
            for o in self.ops[E]:
                for ev in o['waits']:
                    nw += 1
                    if ev[0] == 'e':
                        eng.wait_ge(self.esem[ev[1]], pref[ev[1]][ev[2]])
                    else:
                        eng.wait_ge(self.dsem[ev[1]], ev[2])
                if o['fn'] is None:
                    continue
                if o['fn'] == 'nop':
                    inst = eng.nop()
                else:
                    inst = o['fn'](eng)
                if o['signal']:
                    inst.then_inc(self.esem[E], 1)
                if o['dma'] is not None:
                    inst.then_inc(self.dsem[o['dma']], 16)
            stats[E] = (len(self.ops[E]), nw)

        with nc.Block() as block:
            @block.tensor
            def _(eng):
                run('pe', eng)

            @block.scalar
            def _(eng):
                run('act', eng)

            @block.vector
            def _(eng):
                run('dve', eng)

            @block.gpsimd
            def _(eng):
                run('pool', eng)

            @block.sync
            def _(eng):
                run('sp', eng)
        return stats


class Buf:
    __slots__ = ('t', 'r')

    def __init__(self, t, r):
        self.t = t
        self.r = r

    def __getitem__(self, k):
        return self.t[k]


class _Stop(Exception):
    pass


def build_program(n_exp_decl=NEXP, dbg=False, n_pre=NPRE, n_main=NT, stop=None, sub=99):
    nc = bass.Bass("TRN2", target_bir_lowering=False)

    def din(name, shape, dt=F32):
        return nc.dram_tensor(name, list(shape), dt, kind="ExternalInput").ap()

    x_main = din("x_main", [TOK, D])
    x_pre = din("x_pre", [NPRE * T, D])
    valid_d = din("valid", [T, NPRE])
    pos_d = din("pos", [T, NT + 1], I32)
    mask0_d = din("mask0", [T, 256])
    c_col_d = din("c_col", [T, KC])
    w_ada_d = din("w_ada", [D, 6 * D])
    b_ada_col_d = din("b_ada_col", [T, 48])
    b_ada_d = din("b_ada", [1, 6 * D])
    g_attn_col_d = din("g_attn_col", [T, KC])
    g_ffn_col_d = din("g_ffn_col", [T, KC])
    g_final_d = din("g_final", [1, D])
    w_in_d = din("w_in", [D, IN_W])
    w_gk_up_d = din("w_gk_up", [16, 256])
    b_gk_d = din("b_gk", [1, 256])
    g_gla_d = din("g_gla", [1, 128])
    sinks_d = din("sinks", [1, 8])
    w_out_d = din("w_out", [D, D])
    w_router_d = din("w_router", [D, NEXP])
    rbias_d = din("router_bias", [1, NEXP])
    w_gate_d = din("w_gate", [n_exp_decl, D, 256])
    w_up_d = din("w_up", [n_exp_decl, D, 256])
    w_down_d = din("w_down", [n_exp_decl, 256, D])
    ws_gate_d = din("ws_gate", [D, 256])
    ws_up_d = din("ws_up", [D, 256])
    ws_down_d = din("ws_down", [256, D])
    out_d = nc.dram_tensor("out", [TOK, D], F32, kind="ExternalOutput").ap()
    x1s_d = nc.dram_tensor("x1s", [TOK, D], F32, kind="Internal").ap()
    if dbg:
        d_x1 = nc.dram_tensor("d_x1", [TOK, D], F32, kind="ExternalOutput").ap()
        d_mix = nc.dram_tensor("d_mix", [TOK, D], F32, kind="ExternalOutput").ap()
        d_W = nc.dram_tensor("d_W", [TOK, NEXP + 1], F32, kind="ExternalOutput").ap()

    inv_freq = [float(np.float32(np.power(np.float32(500000.0), np.float32(-(2 * i) / 16.0)))) for i in range(8)]

    with ExitStack() as es:
        S = Sched(nc, es)

        def sbuf(stack, name, shape, dt):
            return Buf(stack.enter_context(nc.sbuf_tensor("sb_" + name, list(shape), dt)), Res(name))

        def A(eng, fn, r=(), w=(), dma=False):
            S.op(eng, fn, reads=[b.r for b in r], writes=[b.r for b in w], dma=dma)

        PB = [Buf(es.enter_context(nc.psum_tensor("pb%d" % i, [128, 512], F32)), Res("pb%d" % i, excl=True)) for i in range(8)]

        h2T = sbuf(es, "h2T", [128, KC, TOK], BF16)
        Wr = sbuf(es, "Wr", [128, NT, NEXP + 1], F32)
        gt1_b = sbuf(es, "gt1_b", [128, D], F32)
        gt2_b = sbuf(es, "gt2_b", [128, D], F32)
        gfin_b = sbuf(es, "gfin_b", [128, D], F32)
        identf = sbuf(es, "identf", [128, 128], F32)
        ident = sbuf(es, "ident", [128, 128], BF16)
        triT = sbuf(es, "triT", [128, 128], F32)
        ustr = sbuf(es, "ustr", [128, 128], F32)
        ones_f = sbuf(es, "ones_f", [128, 128], F32)
        maskb = sbuf(es, "maskb", [128, 256], F32)
        mask0 = sbuf(es, "mask0", [128, 256], F32)
        modcol = sbuf(es, "modcol", [128, 32], F32)
        s1c = sbuf(es, "s1c", [128, KC], F32)
        s2c = sbuf(es, "s2c", [128, KC], F32)
        rbias_b = sbuf(es, "rbias_b", [128, NEXP], F32)
        ggla_b = sbuf(es, "ggla_b", [128, 512], F32)
        sink_b = sbuf(es, "sink_b", [128, 8], F32)
        cosA = sbuf(es, "cosA", [128, NT + 1, 8], F32)
        sinA = sbuf(es, "sinA", [128, NT + 1, 8], F32)
        valid = sbuf(es, "valid", [128, NPRE], F32)

        try:
            A('pool', lambda e: e.memset(ones_f[:], 1.0), w=[ones_f])
            A('pool', lambda e: e.memset(identf[:], 1.0), w=[identf])
            A('pool', lambda e: e.affine_select(out=identf[:], in_=identf[:], pattern=[[1, 128]], compare_op=ALU.is_equal, fill=0.0, base=0, channel_multiplier=-1), r=[identf], w=[identf])
            A('dve', lambda e: e.tensor_copy(out=ident[:], in_=identf[:]), r=[identf], w=[ident])
            A('pool', lambda e: e.affine_select(out=triT[:], in_=ones_f[:], pattern=[[1, 128]], compare_op=ALU.is_ge, fill=0.0, base=0, channel_multiplier=-1), r=[ones_f], w=[triT])
            A('pool', lambda e: e.affine_select(out=ustr[:], in_=ones_f[:], pattern=[[-1, 128]], compare_op=ALU.is_ge, fill=0.0, base=-1, channel_multiplier=1), r=[ones_f], w=[ustr])
            A('pool', lambda e: e.memset(maskb[:], 0.0), w=[maskb])
            A('pool', lambda e: e.affine_select(out=maskb[:, 0:128], in_=maskb[:, 0:128], pattern=[[1, 128]], compare_op=ALU.is_ge, fill=NEG, base=-1, channel_multiplier=-1), r=[maskb], w=[maskb])
            A('pool', lambda e: e.affine_select(out=maskb[:, 128:256], in_=maskb[:, 128:256], pattern=[[-1, 128]], compare_op=ALU.is_ge, fill=NEG, base=0, channel_multiplier=1), r=[maskb], w=[maskb])
            A('sp', lambda e: e.dma_start(out=mask0[:], in_=mask0_d), w=[mask0], dma=True)
            A('sp', lambda e: e.dma_start(out=valid[:], in_=valid_d), w=[valid], dma=True)
            A('sp', lambda e: e.dma_start(out=rbias_b[:], in_=rbias_d.partition_broadcast(128)), w=[rbias_b], dma=True)
            A('sp', lambda e: e.dma_start(out=sink_b[:], in_=sinks_d.partition_broadcast(128)), w=[sink_b], dma=True)
            A('sp', lambda e: e.dma_start(out=gfin_b[:], in_=g_final_d.partition_broadcast(128)), w=[gfin_b], dma=True)
            for h in range(4):
                A('sp', lambda e, h=h: e.dma_start(out=ggla_b[:, h * 128:(h + 1) * 128], in_=g_gla_d.partition_broadcast(128)), w=[ggla_b], dma=True)
            A('sp', lambda e: e.dma_start(out=gt1_b[:], in_=b_ada_d[:, 2 * D:3 * D].partition_broadcast(128)), w=[gt1_b], dma=True)
            A('sp', lambda e: e.dma_start(out=gt2_b[:], in_=b_ada_d[:, 5 * D:6 * D].partition_broadcast(128)), w=[gt2_b], dma=True)
            A('pool', lambda e: e.memset(Wr[:, :, NEXP:NEXP + 1], 1.0), w=[Wr])
            hmk = sbuf(es, 'hmk', [128, 4], F32)
            A('pool', lambda e: e.memset(hmk[:], 0.0), w=[hmk])
            A('pool', lambda e: e.memset(hmk[0:64, 0:1], 1.0), r=[hmk], w=[hmk])
            A('pool', lambda e: e.memset(hmk[64:128, 1:2], 1.0), r=[hmk], w=[hmk])
            A('pool', lambda e: e.memset(hmk[0:64, 2:3], 0.125), r=[hmk], w=[hmk])
            A('pool', lambda e: e.memset(hmk[64:128, 3:4], 0.125), r=[hmk], w=[hmk])

            if stop == 'const':
                raise _Stop()
            with ExitStack() as p0:
                posi = sbuf(p0, "posi", [128, NT + 1], I32)
                posf = sbuf(p0, "posf", [128, NT + 1], F32)
                ang = sbuf(p0, "ang", [128, 2, NT + 1, 8], F32)
                kf = sbuf(p0, "kf", [128, 2, NT + 1, 8], F32)
                ki = sbuf(p0, "ki", [128, 2, NT + 1, 8], I32)
                A('sp', lambda e: e.dma_start(out=posi[:], in_=pos_d), w=[posi], dma=True)
                A('dve', lambda e: e.tensor_copy(out=posf[:], in_=posi[:]), r=[posi], w=[posf])
                for f in range(8):
                    A('dve', lambda e, f=f: e.tensor_scalar(out=ang[:, 0, :, f], in0=posf[:], scalar1=inv_freq[f], scalar2=None, op0=ALU.mult), r=[posf], w=[ang])
                A('dve', lambda e: e.tensor_scalar(out=ang[:, 1], in0=ang[:, 0], scalar1=math.pi / 2, scalar2=None, op0=ALU.add), r=[ang], w=[ang])
                A('dve', lambda e: e.tensor_scalar(out=kf[:], in0=ang[:], scalar1=1.0 / (2 * math.pi), scalar2=None, op0=ALU.mult), r=[ang], w=[kf])
                A('dve', lambda e: e.tensor_copy(out=ki[:], in_=kf[:]), r=[kf], w=[ki])
                A('dve', lambda e: e.tensor_copy(out=kf[:], in_=ki[:]), r=[ki], w=[kf])
                A('dve', lambda e: e.scalar_tensor_tensor(out=ang[:], in0=kf[:], scalar=-2 * math.pi, in1=ang[:], op0=ALU.mult, op1=ALU.add), r=[kf, ang], w=[ang])
                A('dve', lambda e: e.tensor_single_scalar(out=kf[:], in_=ang[:], scalar=math.pi, op=ALU.is_gt), r=[ang], w=[kf])
                A('dve', lambda e: e.scalar_tensor_tensor(out=ang[:], in0=kf[:], scalar=-2 * math.pi, in1=ang[:], op0=ALU.mult, op1=ALU.add), r=[kf, ang], w=[ang])
                A('dve', lambda e: e.tensor_single_scalar(out=kf[:], in_=ang[:], scalar=-math.pi, op=ALU.is_lt), r=[ang], w=[kf])
                A('dve', lambda e: e.scalar_tensor_tensor(out=ang[:], in0=kf[:], scalar=2 * math.pi, in1=ang[:], op0=ALU.mult, op1=ALU.add), r=[kf, ang], w=[ang])
                A('act', lambda e: e.activation(out=sinA[:], in_=ang[:, 0], func=AF.Sin), r=[ang], w=[sinA])
                A('act', lambda e: e.activation(out=cosA[:], in_=ang[:, 1], func=AF.Sin), r=[ang], w=[cosA])

                if stop == 'rope':
                    raise _Stop()
                c_col = sbuf(p0, "c_col", [128, KC], F32)
                cact = sbuf(p0, "cact", [128, KC], F32)
                crep = sbuf(p0, "crep", [128, KC, 128], F32)
                bcol = sbuf(p0, "bcol", [128, 48], F32)
                gac = sbuf(p0, "gac", [128, KC], F32)
                gfc = sbuf(p0, "gfc", [128, KC], F32)
                wa = [sbuf(p0, "wa%d" % i, [128, KC, 512], F32) for i in range(2)]
                A('sp', lambda e: e.dma_start(out=c_col[:], in_=c_col_d), w=[c_col], dma=True)
                A('sp', lambda e: e.dma_start(out=bcol[:], in_=b_ada_col_d), w=[bcol], dma=True)
                A('sp', lambda e: e.dma_start(out=gac[:], in_=g_attn_col_d), w=[gac], dma=True)
                A('sp', lambda e: e.dma_start(out=gfc[:], in_=g_ffn_col_d), w=[gfc], dma=True)
                A('act', lambda e: e.activation(out=cact[:], in_=c_col[:], func=AF.Silu), r=[c_col], w=[cact])
                for k in range(KC):
                    A('dve', lambda e, k=k: e.tensor_scalar(out=crep[:, k, :], in0=ones_f[:], scalar1=cact[:, k:k + 1], scalar2=None, op0=ALU.mult), r=[ones_f, cact], w=[crep])
                colq = 0
                for j in range(12):
                    wb = wa[j % 2]
                    A('sp', lambda e, j=j, wb=wb: e.dma_start(out=wb[:], in_=w_ada_d[:, j * 512:(j + 1) * 512].rearrange("(c p) f -> p c f", p=128)), w=[wb], dma=True)
                    if j in (4, 5, 10, 11):
                        dst = gt1_b if j in (4, 5) else gt2_b
                        half = j % 2
                        pbk = PB[1 + half]
                        for k in range(KC):
                            A('pe', lambda e, k=k, wb=wb, pbk=pbk: e.matmul(pbk[:], lhsT=crep[:, k, :], rhs=wb[:, k, :], start=(k == 0), stop=(k == KC - 1)), r=[crep, wb], w=[pbk])
                        A('dve', lambda e, dst=dst, half=half, pbk=pbk: e.tensor_tensor(out=dst[:, half * 512:(half + 1) * 512], in0=pbk[:], in1=dst[:, half * 512:(half + 1) * 512], op=ALU.add), r=[pbk, dst], w=[dst])
                    else:
                        for q in range(4):
                            for k in range(KC):
                                A('pe', lambda e, k=k, q=q, wb=wb, cq=colq: e.matmul(PB[0][:, cq:cq + 1], lhsT=wb[:, k, q * 128:(q + 1) * 128], rhs=cact[:, k:k + 1], start=(k == 0), stop=(k == KC - 1)), r=[cact, wb], w=[PB[0]])
                            colq += 1
                A('dve', lambda e: e.tensor_tensor(out=modcol[:, 0:16], in0=PB[0][:, 0:16], in1=bcol[:, 0:16], op=ALU.add), r=[PB[0], bcol], w=[modcol])
                A('dve', lambda e: e.tensor_tensor(out=modcol[:, 16:32], in0=PB[0][:, 16:32], in1=bcol[:, 24:40], op=ALU.add), r=[PB[0], bcol], w=[modcol])
                A('dve', lambda e: e.scalar_tensor_tensor(out=s1c[:], in0=modcol[:, 8:16], scalar=1.0, in1=gac[:], op0=ALU.add, op1=ALU.mult), r=[modcol, gac], w=[s1c])
                A('dve', lambda e: e.scalar_tensor_tensor(out=s2c[:], in0=modcol[:, 24:32], scalar=1.0, in1=gfc[:], op0=ALU.add, op1=ALU.mult), r=[modcol, gfc], w=[s2c])
                S.barrier()
            if stop == 'ada':
                raise _Stop()

            with ExitStack() as p1:
                w_in = sbuf(p1, "w_in", [128, KC, IN_W], BF16)
                w_out = sbuf(p1, "w_out", [128, KC, D], BF16)
                w_rt = sbuf(p1, "w_rt", [128, KC, NEXP], BF16)
                wgk = sbuf(p1, "wgk", [16, 256], F32)
                bgk = sbuf(p1, "bgk", [1, 256], F32)
                stg = sbuf(p1, "stg", [128, IN_W], F32)
                for k in range(KC):
                    A('sp', lambda e, k=k: e.dma_start(out=stg[:], in_=w_in_d[k * 128:(k + 1) * 128, :]), w=[stg], dma=True)
                    A('pool', lambda e, k=k: e.tensor_copy(out=w_in[:, k, :], in_=stg[:]), r=[stg], w=[w_in])
                for k in range(KC):
                    A('sp', lambda e, k=k: e.dma_start(out=stg[:, 0:D], in_=w_out_d[k * 128:(k + 1) * 128, :]), w=[stg], dma=True)
                    A('pool', lambda e, k=k: e.tensor_copy(out=w_out[:, k, :], in_=stg[:, 0:D]), r=[stg], w=[w_out])
                A('sp', lambda e: e.dma_start(out=stg[:, 0:KC * NEXP].rearrange("p (c f) -> p c f", c=KC), in_=w_router_d.rearrange("(c p) f -> p c f", p=128)), w=[stg], dma=True)
                A('pool', lambda e: e.tensor_copy(out=w_rt[:].rearrange("p c f -> p (c f)"), in_=stg[:, 0:KC * NEXP]), r=[stg], w=[w_rt])
                A('sp', lambda e: e.dma_start(out=wgk[:], in_=w_gk_up_d), w=[wgk], dma=True)
                A('sp', lambda e: e.dma_start(out=bgk[:], in_=b_gk_d), w=[bgk], dma=True)

                xts = [sbuf(p1, "xt%d" % i, [128, D], F32) for i in range(2)]
                xtc = [0]
                sqj = sbuf(p1, "sqj", [128, D], F32)
                ss = sbuf(p1, "ss", [128, 4], F32)
                xs = sbuf(p1, "xs", [128, D], BF16)
                hT = sbuf(p1, "hT", [128, KC, 128], BF16)
                glowT = sbuf(p1, "glowT", [16, 128], F32)
                ab = sbuf(p1, "ab", [128, 256], F32)
                ex = sbuf(p1, "ex", [128, 256], F32)
                la = sbuf(p1, "la", [128, 256], F32)
                E1 = sbuf(p1, "E1", [128, 256], F32)
                E2 = sbuf(p1, "E2", [128, 256], F32)
                E3 = sbuf(p1, "E3", [128, 256], F32)
                decc = sbuf(p1, "decc", [128, 2], F32)
                qdT = sbuf(p1, "qdT", [128, 4, 128], BF16)
                kdT = sbuf(p1, "kdT", [128, 4, 128], BF16)
                kdec = sbuf(p1, "kdec", [128, 256], BF16)
                v_bf = sbuf(p1, "v_bf", [128, 512], BF16)
                attnT = sbuf(p1, "attnT", [128, 4, 128], BF16)
                Sst = sbuf(p1, "Sst", [128, 2, 128], F32)
                S_bf = sbuf(p1, "S_bf", [128, 2, 128], BF16)
                ssg = sbuf(p1, "ssg", [128, 4], F32)
                rstdg = sbuf(p1, "rstdg", [128, 4], F32)
                gsil = sbuf(p1, "gsil", [128, 512], F32)
                mix = sbuf(p1, "mix", [128, D], BF16)
                mixT = sbuf(p1, "mixT", [128, KC, 128], BF16)
                q_r = sbuf(p1, "q_r", [128, 4, 2, 64], BF16)
                rt1 = sbuf(p1, "rt1", [128, 4, 2, 8], F32)
                rt2 = sbuf(p1, "rt2", [128, 4, 2, 8], F32)
                kv_r = [sbuf(p1, "kv_r%d" % i, [128, 256], BF16) for i in range(2)]
                kT = [sbuf(p1, "kT%d" % i, [128, 128], BF16) for i in range(2)]
                qT = sbuf(p1, "qT", [128, 2, 4, 128], BF16)
                sm = sbuf(p1, "sm", [128, 4, 256], F32)
                rmax = sbuf(p1, "rmax", [128, 8], F32)
                negm = sbuf(p1, "negm", [128, 8], F32)
                rsum = sbuf(p1, "rsum", [128, 8], F32)
                esk = sbuf(p1, "esk", [128, 8], F32)
                pb = sbuf(p1, "pb", [128, 4, 256], BF16)
                pT = sbuf(p1, "pT", [128, 8, 128], BF16)
                x1 = sbuf(p1, "x1", [128, D], F32)
                tmpm = sbuf(p1, "tmpm", [128, D], F32)
                sc = sbuf(p1, "sc", [128, NEXP], F32)
                sel = sbuf(p1, "sel", [128, NEXP], F32)
                sel2 = sbuf(p1, "sel2", [128, NEXP], F32)
                eqm = sbuf(p1, "eqm", [128, NEXP], F32)
                m1 = sbuf(p1, "m1", [128, 8], F32)
                m2 = sbuf(p1, "m2", [128, 8], F32)
                gs = sbuf(p1, "gs", [128, 8], F32)
                top8 = sbuf(p1, "top8", [128, 8], F32)
                pen = sbuf(p1, "pen", [128, 8], F32)
                wsum = sbuf(p1, "wsum", [128, 1], F32)

                A('dve', lambda e: e.memset(Sst[:], 0.0), w=[Sst])
                A('dve', lambda e: e.memset(S_bf[:], 0.0), w=[S_bf])

                def bfv(pbuf):
                    return pbuf[:].bitcast(BF16)

                def front(xsrc, scol, bcol_, hT_dst):
                    xt = xts[xtc[0] % 2]
                    xtc[0] += 1
                    A('sp', lambda e: e.dma_start(out=xt[:], in_=xsrc), w=[xt], dma=True)
                    front_from_sbuf(xt, scol, bcol_, hT_dst)
                    return xt

                def front_from_sbuf(xsb, scol, bcol_, hT_dst, hT_buf=None):
                    hb = hT if hT_buf is None else hT_buf
                    A('act', lambda e: e.activation(out=sqj[:], in_=xsb[:], func=AF.Square, accum_out=ss[:, 0:1]), r=[xsb], w=[sqj, ss])
                    A('act', lambda e: e.activation(out=ss[:, 1:2], in_=ss[:, 0:1], func=AF.Sqrt, scale=1.0 / D, bias=EPS), r=[ss], w=[ss])
                    A('dve', lambda e: e.reciprocal(out=ss[:, 2:3], in_=ss[:, 1:2]), r=[ss], w=[ss])
                    A('dve', lambda e: e.tensor_scalar(out=xs[:], in0=xsb[:], scalar1=ss[:, 2:3], scalar2=None, op0=ALU.mult), r=[xsb, ss], w=[xs])
                    pv = bfv(PB[0])
                    for c in range(KC):
                        A('pe', lambda e, c=c: e.transpose(out=pv[:, c * 128:(c + 1) * 128], in_=xs[:, c * 128:(c + 1) * 128], identity=ident[:]), r=[xs, ident], w=[PB[0]])
                    for c in range(KC):
                        A('act', lambda e, c=c: e.activation(out=hT_dst(c), in_=pv[:, c * 128:(c + 1) * 128], func=AF.Identity, scale=scol[:, c:c + 1], bias=bcol_[:, c:c + 1]), r=[PB[0], scol, bcol_], w=[hb])

                def proj_tok(pbk, col0, col1, c_lo, c_hi):
                    n = c_hi - c_lo
                    for k in range(KC):
                        A('pe', lambda e, k=k: e.matmul(pbk[:, col0:col0 + n], lhsT=hT[:, k, :], rhs=w_in[:, k, c_lo:c_hi], start=(k == 0), stop=(k == KC - 1)), r=[hT, w_in], w=[pbk])

                def proj_feat(pbk, col0, c_lo, m):
                    for k in range(KC):
                        A('pe', lambda e, k=k: e.matmul(pbk[0:m, col0:col0 + 128], lhsT=w_in[:, k, c_lo:c_lo + m], rhs=hT[:, k, :], start=(k == 0), stop=(k == KC - 1)), r=[hT, w_in], w=[pbk])

                def gate_logs():
                    A('dve', lambda e: e.tensor_copy(out=glowT[:], in_=PB[6][0:16, 0:128]), r=[PB[6]], w=[glowT])
                    A('pe', lambda e: e.matmul(PB[6][:, 128:384], lhsT=glowT[:], rhs=wgk[:], start=True, stop=False), r=[glowT, wgk], w=[PB[6]])
                    A('pe', lambda e: e.matmul(PB[6][:, 128:384], lhsT=ones_f[0:1, :], rhs=bgk[:], start=False, stop=True), r=[ones_f, bgk], w=[PB[6]])
                    pre = PB[6][:, 128:384]
                    A('act', lambda e: e.activation(out=ab[:], in_=pre, func=AF.Abs), r=[PB[6]], w=[ab])
                    A('act', lambda e: e.activation(out=ex[:], in_=ab[:], func=AF.Exp, scale=-1.0), r=[ab], w=[ex])
                    A('act', lambda e: e.activation(out=ex[:], in_=ex[:], func=AF.Ln, bias=1.0), r=[ex], w=[ex])
                    A('dve', lambda e: e.scalar_tensor_tensor(out=la[:], in0=pre, scalar=0.0, in1=ex[:], op0=ALU.min, op1=ALU.subtract), r=[PB[6], ex], w=[la])
                    A('dve', lambda e: e.tensor_scalar(out=la[:], in0=la[:], scalar1=1.0 / 16.0, scalar2=None, op0=ALU.mult), r=[la], w=[la])

                def state_update():
                    for p in range(2):
                        A('pe', lambda e, p=p: e.matmul(PB[7][:, p * 256:(p + 1) * 256], lhsT=kdec[:, p * 128:(p + 1) * 128], rhs=v_bf[:, p * 256:(p + 1) * 256], start=True, stop=True), r=[kdec, v_bf], w=[PB[7]])
                    for p in range(2):
                        for w_ in range(2):
                            rs = slice(w_ * 64, (w_ + 1) * 64)
                            A('dve', lambda e, p=p, w_=w_, rs=rs: e.scalar_tensor_tensor(out=Sst[rs, p, :], in0=Sst[rs, p, :], scalar=decc[rs, p:p + 1], in1=PB[7][rs, p * 256 + w_ * 128:p * 256 + (w_ + 1) * 128], op0=ALU.mult, op1=ALU.add), r=[Sst, decc, PB[7]], w=[Sst])
                    A('act', lambda e: e.copy(out=S_bf[:], in_=Sst[:]), r=[Sst], w=[S_bf])

                def prefix_tile(t, last):
                    front(x_pre[t * 128:(t + 1) * 128, :], s1c, modcol_sh1, lambda c: hT[:, c, :])
                    if sub < 2:
                        return
                    proj_tok(PB[1], 0, 512, C_GV, C_GV + 512)
                    if last:
                        proj_tok(PB[4], 0, 256, C_SK, C_SK + 256)
                        proj_tok(PB[4], 256, 512, C_GK, C_GK + 256)
                    else:
                        proj_tok(PB[4], 256, 512, C_GK, C_GK + 256)
                    proj_feat(PB[6], 0, C_GLOW, 16)
                    if sub < 3:
                        return
                    gate_logs()
                    if sub < 4:
                        return
                    A('pe', lambda e: e.matmul(PB[7][:, 256:512], lhsT=ustr[:], rhs=la[:], start=True, stop=True), r=[ustr, la], w=[PB[7]])
                    for p in range(2):
                        A('pe', lambda e, p=p: e.matmul(PB[7][:, p:p + 1], lhsT=la[:, p * 128:(p + 1) * 128], rhs=ones_f[:, 0:1], start=True, stop=True), r=[la, ones_f], w=[PB[7]])
                    A('act', lambda e: e.activation(out=E3[:], in_=PB[7][:, 256:512], func=AF.Exp), r=[PB[7]], w=[E3])
                    A('act', lambda e: e.activation(out=decc[:], in_=PB[7][:, 0:2], func=AF.Exp), r=[PB[7]], w=[decc])
                    A('dve', lambda e: e.scalar_tensor_tensor(out=kdec[:], in0=PB[4][:, 256:512], scalar=valid[:, t:t + 1], in1=E3[:], op0=ALU.mult, op1=ALU.mult), r=[PB[4], valid, E3], w=[kdec])
                    A('act', lambda e: e.copy(out=v_bf[:], in_=PB[1][:]), r=[PB[1]], w=[v_bf])
                    if sub < 5:
                        return
                    if last:
                        rope_kv(0, 0)
                    if sub < 6:
                        return
                    state_update()

                def rope_kv(slot, tcol):
                    kvb = kv_r[slot]
                    A('dve', lambda e: e.tensor_copy(out=kvb[:], in_=PB[4][:, 0:256]), r=[PB[4]], w=[kvb])
                    kv3 = PB[4][:, 0:128].rearrange("p (h d) -> p h d", h=2)
                    ko3 = kvb[:, 0:128].rearrange("p (h d) -> p h d", h=2)
                    cb = cosA[:, tcol, :].unsqueeze(1).to_broadcast([128, 2, 8])
                    sb_ = sinA[:, tcol, :].unsqueeze(1).to_broadcast([128, 2, 8])
                    r1 = rt1[:, 0, :, :]
                    r2 = rt2[:, 0, :, :]
                    RD = int(os.environ.get('ROPE_DBG', '9'))
                    if RD > 0:
                        A('dve', lambda e: e.tensor_tensor(out=r1, in0=kv3[:, :, 0:8], in1=cb, op=ALU.mult), r=[PB[4], cosA], w=[rt1])
                    if RD > 1:
                        A('dve', lambda e: e.tensor_tensor(out=r2, in0=kv3[:, :, 8:16], in1=sb_, op=ALU.mult), r=[PB[4], sinA], w=[rt2])
                    if RD > 2:
                        A('dve', lambda e: e.tensor_tensor(out=ko3[:, :, 0:8], in0=r1, in1=r2, op=ALU.subtract), r=[rt1, rt2], w=[kvb])
                    if RD > 3:
                        A('dve', lambda e: e.tensor_tensor(out=r1, in0=kv3[:, :, 8:16], in1=cb, op=ALU.mult), r=[PB[4], cosA], w=[rt1])
                    if RD > 4:
                        A('dve', lambda e: e.tensor_tensor(out=r2, in0=kv3[:, :, 0:8], in1=sb_, op=ALU.mult), r=[PB[4], sinA], w=[rt2])
                    if RD > 5:
                        A('dve', lambda e: e.tensor_tensor(out=ko3[:, :, 8:16], in0=r1, in1=r2, op=ALU.add), r=[rt1, rt2], w=[kvb])
                    pv = bfv(PB[0])
                    if RD > -2:
                        A('pe', lambda e: e.transpose(out=pv[:, 0:128], in_=kvb[:, 0:128], identity=ident[:]), r=[kvb, ident], w=[PB[0]])
                    if RD > -1:
                        A('act', lambda e: e.copy(out=kT[slot][:], in_=pv[:, 0:128]), r=[PB[0]], w=[kT[slot]])

                modcol_sh1 = Buf(modcol.t[:, 0:8], modcol.r)
                modcol_sh2 = Buf(modcol.t[:, 16:24], modcol.r)

                for t in range(NPRE - n_pre, NPRE):
                    prefix_tile(t, t == NPRE - 1)

                def main_tile(i):
                    cur = (i + 1) % 2
                    prv = i % 2
                    tcol = i + 1
                    xt = front(x_main[i * 128:(i + 1) * 128, :], s1c, modcol_sh1, lambda c: hT[:, c, :])
                    proj_tok(PB[1], 0, 512, C_GV, C_GV + 512)
                    SKIPP = int(os.environ.get('SKIPP', '0'))
                    if not SKIPP & 1:
                        proj_tok(PB[2], 0, 512, C_GR, C_GR + 512)
                    if not SKIPP & 2:
                        proj_tok(PB[3], 0, 512, C_SQ, C_SQ + 512)
                    proj_tok(PB[4], 0, 256, C_SK, C_SK + 256)
                    proj_tok(PB[4], 256, 512, C_GK, C_GK + 256)
                    if not SKIPP & 4:
                        for p in range(2):
                            proj_feat(PB[5], p * 128, C_GQ + p * 128, 128)
                        for p in range(2):
                            proj_feat(PB[5], 256 + p * 128, C_GK + p * 128, 128)
                    proj_feat(PB[6], 0, C_GLOW, 16)
                    if sub < 10:
                        return
                    gate_logs()
                    XS = int(os.environ.get('XS', '0'))
                    if not XS & 1:
                        for p in range(2):
                            A('pe', lambda e, p=p: e.matmul(PB[7][:, p * 128:(p + 1) * 128], lhsT=la[:, p * 128:(p + 1) * 128], rhs=triT[:], start=True, stop=True), r=[la, triT], w=[PB[7]])
                    A('pe', lambda e: e.matmul(PB[7][:, 256:512], lhsT=ustr[:], rhs=la[:], start=True, stop=True), r=[ustr, la], w=[PB[7]])
                    if not XS & 2:
                        A('act', lambda e: e.activation(out=E1[:], in_=PB[7][:, 0:256], func=AF.Exp), r=[PB[7]], w=[E1])
                    if not XS & 4:
                        A('act', lambda e: e.activation(out=E2[:], in_=PB[7][:, 0:256], func=AF.Exp, scale=-1.0), r=[PB[7]], w=[E2])
                    A('act', lambda e: e.activation(out=E3[:], in_=PB[7][:, 256:512], func=AF.Exp), r=[PB[7]], w=[E3])
                    if sub < 11:
                        return
                    MS = int(os.environ.get('MS', '0'))
                    if not (MS >> 0) & 1:
                        A('dve', lambda e: e.tensor_copy(out=decc[:, 0:1], in_=E1[:, 127:128]), r=[E1], w=[decc])
                    if not (MS >> 1) & 1:
                        A('dve', lambda e: e.tensor_copy(out=decc[:, 1:2], in_=E1[:, 255:256]), r=[E1], w=[decc])
                    if not (MS >> 2) & 1:
                        for h_ in range(4):
                            p_, w__ = h_ // 2, h_ % 2
                            A('dve', lambda e, h_=h_, p_=p_, w__=w__: e.scalar_tensor_tensor(out=qdT[:, h_, :], in0=PB[5][:, p_ * 128:(p_ + 1) * 128], scalar=hmk[:, 2 + w__:3 + w__], in1=E1[:, p_ * 128:(p_ + 1) * 128], op0=ALU.mult, op1=ALU.mult), r=[PB[5], E1, hmk], w=[qdT])
                    if not (MS >> 3) & 1:
                        for h_ in range(4):
                            p_, w__ = h_ // 2, h_ % 2
                            A('dve', lambda e, h_=h_, p_=p_, w__=w__: e.scalar_tensor_tensor(out=kdT[:, h_, :], in0=PB[5][:, 256 + p_ * 128:256 + (p_ + 1) * 128], scalar=hmk[:, w__:w__ + 1], in1=E2[:, p_ * 128:(p_ + 1) * 128], op0=ALU.mult, op1=ALU.mult), r=[PB[5], E2, hmk], w=[kdT])
                    if not (MS >> 4) & 1:
                        A('dve', lambda e: e.scalar_tensor_tensor(out=kdec[:], in0=PB[4][:, 256:512], scalar=1.0, in1=E3[:], op0=ALU.mult, op1=ALU.mult), r=[PB[4], E3], w=[kdec])
                    VB = int(os.environ.get('VB', '0'))
                    if (MS >> 5) & 1:
                        pass
                    elif VB == 1:
                        A('act', lambda e: e.copy(out=gsil[:], in_=PB[1][:]), r=[PB[1]], w=[gsil])
                    elif VB == 2:
                        A('dve', lambda e: e.tensor_copy(out=v_bf[:], in_=PB[1][:]), r=[PB[1]], w=[v_bf])
                    else:
                        A('act', lambda e: e.copy(out=v_bf[:], in_=PB[1][:]), r=[PB[1]], w=[v_bf])
                    if sub < 12:
                        return
                    for h in range(4):
                        p, w_ = h // 2, h % 2
                        rs = slice(w_ * 64, (w_ + 1) * 64)
                        A('pe', lambda e, h=h, p=p, rs=rs: e.matmul(PB[5][:, h * 128:(h + 1) * 128], lhsT=kdT[:, h, :], rhs=qdT[:, h, :], start=True, stop=True), r=[kdT, qdT], w=[PB[5]])
                    for h in range(4):
                        A('dve', lambda e, h=h: e.scalar_tensor_tensor(out=attnT[:, h, :], in0=PB[5][:, h * 128:(h + 1) * 128], scalar=1.0, in1=triT[:], op0=ALU.mult, op1=ALU.mult), r=[PB[5], triT], w=[attnT])
                    if sub < 13:
                        return
                    for h in range(4):
                        p, w_ = h // 2, h % 2
                        rs = slice(w_ * 64, (w_ + 1) * 64)
                        A('pe', lambda e, h=h: e.matmul(PB[1][:, h * 128:(h + 1) * 128], lhsT=attnT[:, h, :], rhs=v_bf[:, h * 128:(h + 1) * 128], start=True, stop=False), r=[attnT, v_bf], w=[PB[1]])
                        A('pe', lambda e, h=h, p=p, rs=rs: e.matmul(PB[1][:, h * 128:(h + 1) * 128], lhsT=qdT[:, h, :], rhs=S_bf[:, p, :], start=False, stop=True), r=[qdT, S_bf], w=[PB[1]])
                    state_update()
                    if sub < 14:
                        return
                    for h in range(4):
                        A('act', lambda e, h=h: e.activation(out=sqj[:, h * 128:(h + 1) * 128], in_=PB[1][:, h * 128:(h + 1) * 128], func=AF.Square, accum_out=ssg[:, h:h + 1]), r=[PB[1]], w=[sqj, ssg])
                    A('act', lambda e: e.activation(out=ssg[:], in_=ssg[:], func=AF.Sqrt, scale=1.0 / 128, bias=EPS), r=[ssg], w=[ssg])
                    A('dve', lambda e: e.reciprocal(out=rstdg[:], in_=ssg[:]), r=[ssg], w=[rstdg])
                    A('act', lambda e: e.activation(out=gsil[:], in_=PB[2][:], func=AF.Silu), r=[PB[2]], w=[gsil])
                    A('dve', lambda e: e.tensor_tensor(out=gsil[:], in0=gsil[:], in1=ggla_b[:], op=ALU.mult), r=[gsil, ggla_b], w=[gsil])
                    for h in range(4):
                        A('dve', lambda e, h=h: e.scalar_tensor_tensor(out=mix[:, h * 128:(h + 1) * 128], in0=PB[1][:, h * 128:(h + 1) * 128], scalar=rstdg[:, h:h + 1], in1=gsil[:, h * 128:(h + 1) * 128], op0=ALU.mult, op1=ALU.mult), r=[PB[1], rstdg, gsil], w=[mix])
                    if sub < 15:
                        return
                    rope_kv(cur, tcol)
                    q4 = PB[3][:].rearrange("p (w a d) -> p a w d", w=2, a=4)
                    A('act', lambda e: e.copy(out=q_r[:], in_=q4), r=[PB[3]], w=[q_r])
                    cb = cosA[:, tcol, :].unsqueeze(1).unsqueeze(1).to_broadcast([128, 4, 2, 8])
                    sb_ = sinA[:, tcol, :].unsqueeze(1).unsqueeze(1).to_broadcast([128, 4, 2, 8])
                    A('dve', lambda e: e.tensor_tensor(out=rt1[:], in0=q4[:, :, :, 0:8], in1=cb, op=ALU.mult), r=[PB[3], cosA], w=[rt1])
                    A('dve', lambda e: e.tensor_tensor(out=rt2[:], in0=q4[:, :, :, 8:16], in1=sb_, op=ALU.mult), r=[PB[3], sinA], w=[rt2])
                    A('dve', lambda e: e.tensor_tensor(out=q_r[:, :, :, 0:8], in0=rt1[:], in1=rt2[:], op=ALU.subtract), r=[rt1, rt2], w=[q_r])
                    A('dve', lambda e: e.tensor_tensor(out=rt1[:], in0=q4[:, :, :, 8:16], in1=cb, op=ALU.mult), r=[PB[3], cosA], w=[rt1])
                    A('dve', lambda e: e.tensor_tensor(out=rt2[:], in0=q4[:, :, :, 0:8], in1=sb_, op=ALU.mult), r=[PB[3], sinA], w=[rt2])
                    A('dve', lambda e: e.tensor_tensor(out=q_r[:, :, :, 8:16], in0=rt1[:], in1=rt2[:], op=ALU.add), r=[rt1, rt2], w=[q_r])
                    if sub < 16:
                        return
                    pv0 = bfv(PB[0])
                    for a in range(4):
                        A('pe', lambda e, a=a: e.transpose(out=pv0[:, a * 128:(a + 1) * 128], in_=q_r[:, a, :, :].rearrange("p w d -> p (w d)"), identity=ident[:]), r=[q_r, ident], w=[PB[0]])
                    for w__ in range(2):
                        A('dve', lambda e, w__=w__: e.tensor_scalar(out=qT[:, w__, :, :].rearrange("p a t -> p (a t)"), in0=pv0[:, 0:512], scalar1=hmk[:, w__:w__ + 1], scalar2=None, op0=ALU.mult), r=[PB[0], hmk], w=[qT])
                    if sub < 17:
                        return
                    mk = mask0 if i == 0 else maskb
                    for g in range(2):
                        banks = (PB[3], PB[4]) if g == 0 else (PB[2], PB[6])
                        for ai in range(2):
                            a = g * 2 + ai
                            bk = banks[ai]
                            for w_ in range(2):
                                rs = slice(w_ * 64, (w_ + 1) * 64)
                                A('pe', lambda e, a=a, w_=w_, rs=rs, bk=bk: e.matmul(bk[:, w_ * 256:w_ * 256 + 128], lhsT=qT[:, w_, a, :], rhs=kT[prv][:, :], start=True, stop=True), r=[qT, kT[prv]], w=[bk])
                                A('pe', lambda e, a=a, w_=w_, rs=rs, bk=bk: e.matmul(bk[:, w_ * 256 + 128:w_ * 256 + 256], lhsT=qT[:, w_, a, :], rhs=kT[cur][:, :], start=True, stop=True), r=[qT, kT[cur]], w=[bk])
                        for ai in range(2):
                            bk = banks[ai]
                            for w_ in range(2):
                                j = ai * 2 + w_
                                A('dve', lambda e, j=j, w_=w_, bk=bk, mk=mk: e.scalar_tensor_tensor(out=sm[:, j, :], in0=bk[:, w_ * 256:(w_ + 1) * 256], scalar=0.125, in1=mk[:], op0=ALU.mult, op1=ALU.add), r=[bk, mk], w=[sm])
                        gc = slice(g * 4, g * 4 + 4)
                        A('dve', lambda e, gc=gc: e.tensor_reduce(out=rmax[:, gc], in_=sm[:], axis=AX.X, op=ALU.max), r=[sm], w=[rmax])
                        for ai in range(2):
                            for w_ in range(2):
                                j = g * 4 + ai * 2 + w_
                                hq = w_ * 4 + g * 2 + ai
                                A('dve', lambda e, j=j, hq=hq: e.tensor_tensor(out=rmax[:, j:j + 1], in0=rmax[:, j:j + 1], in1=sink_b[:, hq:hq + 1], op=ALU.max), r=[rmax, sink_b], w=[rmax])
                                A('dve', lambda e, j=j, hq=hq: e.tensor_tensor(out=esk[:, j:j + 1], in0=sink_b[:, hq:hq + 1], in1=rmax[:, j:j + 1], op=ALU.subtract), r=[rmax, sink_b], w=[esk])
                        A('dve', lambda e, gc=gc: e.tensor_scalar(out=negm[:, gc], in0=rmax[:, gc], scalar1=-1.0, scalar2=None, op0=ALU.mult), r=[rmax], w=[negm])
                        for j in range(4):
                            A('act', lambda e, j=j, g=g: e.activation(out=pb[:, j, :], in_=sm[:, j, :], func=AF.Exp, bias=negm[:, g * 4 + j:g * 4 + j + 1], accum_out=rsum[:, g * 4 + j:g * 4 + j + 1]), r=[sm, negm], w=[pb, rsum])
                        A('act', lambda e, gc=gc: e.activation(out=esk[:, gc], in_=esk[:, gc], func=AF.Exp), r=[esk], w=[esk])
                        A('dve', lambda e, gc=gc: e.tensor_tensor(out=rsum[:, gc], in0=rsum[:, gc], in1=esk[:, gc], op=ALU.add), r=[rsum, esk], w=[rsum])
                        A('dve', lambda e, gc=gc: e.reciprocal(out=rsum[:, gc], in_=rsum[:, gc]), r=[rsum], w=[rsum])
                        tb = PB[5] if g == 0 else PB[7]
                        tv = bfv(tb)
                        for j in range(4):
                            for blk in range(2):
                                A('pe', lambda e, j=j, blk=blk, tv=tv: e.transpose(out=tv[:, (j * 2 + blk) * 128:(j * 2 + blk + 1) * 128], in_=pb[:, j, blk * 128:(blk + 1) * 128], identity=ident[:]), r=[pb, ident], w=[tb])
                        A('act', lambda e, tv=tv: e.copy(out=pT[:].rearrange("p a t -> p (a t)"), in_=tv), r=[tb], w=[pT])
                        for ai in range(2):
                            for w_ in range(2):
                                j = ai * 2 + w_
                                hq = w_ * 4 + g * 2 + ai
                                A('pe', lambda e, j=j, hq=hq, w_=w_: e.matmul(PB[1][:, hq * 64:(hq + 1) * 64], lhsT=pT[:, j * 2, :], rhs=kv_r[prv][:, 128 + w_ * 64:128 + (w_ + 1) * 64], start=True, stop=False), r=[pT, kv_r[prv]], w=[PB[1]])
                                A('pe', lambda e, j=j, hq=hq, w_=w_: e.matmul(PB[1][:, hq * 64:(hq + 1) * 64], lhsT=pT[:, j * 2 + 1, :], rhs=kv_r[cur][:, 128 + w_ * 64:128 + (w_ + 1) * 64], start=False, stop=True), r=[pT, kv_r[cur]], w=[PB[1]])
                        for ai in range(2):
                            for w_ in range(2):
                                j = g * 4 + ai * 2 + w_
                                hq = w_ * 4 + g * 2 + ai
                                A('dve', lambda e, j=j, hq=hq: e.tensor_scalar(out=mix[:, 512 + hq * 64:512 + (hq + 1) * 64], in0=PB[1][:, hq * 64:(hq + 1) * 64], scalar1=rsum[:, j:j + 1], scalar2=None, op0=ALU.mult), r=[PB[1], rsum], w=[mix])
                    if sub < 18:
                        return
                    pv = bfv(PB[0])
                    for c in range(KC):
                        A('pe', lambda e, c=c: e.transpose(out=pv[:, c * 128:(c + 1) * 128], in_=mix[:, c * 128:(c + 1) * 128], identity=ident[:]), r=[mix, ident], w=[PB[0]])
                    A('act', lambda e: e.copy(out=mixT[:].rearrange("p c t -> p (c t)"), in_=pv), r=[PB[0]], w=[mixT])
                    for dh in range(2):
                        bk = PB[4] if dh == 0 else PB[6]
                        for k in range(KC):
                            A('pe', lambda e, k=k, dh=dh, bk=bk: e.matmul(bk[:], lhsT=mixT[:, k, :], rhs=w_out[:, k, dh * 512:(dh + 1) * 512], start=(k == 0), stop=(k == KC - 1)), r=[mixT, w_out], w=[bk])
                        A('dve', lambda e, dh=dh, bk=bk: e.tensor_tensor(out=tmpm[:, dh * 512:(dh + 1) * 512], in0=bk[:], in1=gt1_b[:, dh * 512:(dh + 1) * 512], op=ALU.mult), r=[bk, gt1_b], w=[tmpm])
                    A('dve', lambda e: e.tensor_tensor(out=x1[:], in0=tmpm[:], in1=xt[:], op=ALU.add), r=[tmpm, xt], w=[x1])
                    A('sp', lambda e: e.dma_start(out=x1s_d[i * 128:(i + 1) * 128, :], in_=x1[:]), r=[x1], dma=True)
                    if dbg:
                        A('sp', lambda e: e.dma_start(out=d_x1[i * 128:(i + 1) * 128, :], in_=x1[:]), r=[x1], dma=True)
                        A('dve', lambda e: e.tensor_copy(out=tmpm[:], in_=mix[:]), r=[mix], w=[tmpm])
                        A('sp', lambda e: e.dma_start(out=d_mix[i * 128:(i + 1) * 128, :], in_=tmpm[:]), r=[tmpm], dma=True)
                    if sub < 19:
                        return
                    front_from_sbuf(x1, s2c, modcol_sh2, lambda c: h2T[:, c, i * 128:(i + 1) * 128], hT_buf=h2T)
                    for k in range(KC):
                        A('pe', lambda e, k=k: e.matmul(PB[5][:, 0:NEXP], lhsT=h2T[:, k, i * 128:(i + 1) * 128], rhs=w_rt[:, k, :], start=(k == 0), stop=(k == KC - 1)), r=[h2T, w_rt], w=[PB[5]])
                    if sub < 20:
                        return
                    A('act', lambda e: e.activation(out=sc[:], in_=PB[5][:, 0:NEXP], func=AF.Sigmoid), r=[PB[5]], w=[sc])
                    A('dve', lambda e: e.tensor_tensor(out=sel[:], in0=sc[:], in1=rbias_b[:], op=ALU.add), r=[sc, rbias_b], w=[sel])
                    sel3 = sel[:].rearrange("p (g k) -> p g k", g=8)
                    A('dve', lambda e: e.tensor_reduce(out=m1[:], in_=sel3, axis=AX.X, op=ALU.max), r=[sel], w=[m1])
                    A('dve', lambda e: e.tensor_tensor(out=eqm[:].rearrange("p (g k) -> p g k", g=8), in0=sel3, in1=m1[:].unsqueeze(2).to_broadcast([128, 8, 32]), op=ALU.is_equal), r=[sel, m1], w=[eqm])
                    A('dve', lambda e: e.scalar_tensor_tensor(out=sel2[:], in0=eqm[:], scalar=-1e30, in1=sel[:], op0=ALU.mult, op1=ALU.add), r=[eqm, sel], w=[sel2])
                    A('dve', lambda e: e.tensor_reduce(out=m2[:], in_=sel2[:].rearrange("p (g k) -> p g k", g=8), axis=AX.X, op=ALU.max), r=[sel2], w=[m2])
                    A('dve', lambda e: e.tensor_tensor(out=gs[:], in0=m1[:], in1=m2[:], op=ALU.add), r=[m1, m2], w=[gs])
                    A('dve', lambda e: e.max(out=top8[:], in_=gs[:]), r=[gs], w=[top8])
                    A('dve', lambda e: e.tensor_scalar(out=pen[:], in0=gs[:], scalar1=top8[:, 3:4], scalar2=1e30, op0=ALU.is_lt, op1=ALU.mult), r=[gs, top8], w=[pen])
                    A('dve', lambda e: e.tensor_tensor(out=sel2[:].rearrange("p (g k) -> p g k", g=8), in0=sel3, in1=pen[:].unsqueeze(2).to_broadcast([128, 8, 32]), op=ALU.subtract), r=[sel, pen], w=[sel2])
                    A('dve', lambda e: e.max(out=top8[:], in_=sel2[:]), r=[sel2], w=[top8])
                    A('dve', lambda e: e.tensor_scalar(out=eqm[:], in0=sel2[:], scalar1=top8[:, 7:8], scalar2=None, op0=ALU.is_ge), r=[sel2, top8], w=[eqm])
                    A('dve', lambda e: e.tensor_tensor(out=eqm[:], in0=eqm[:], in1=sc[:], op=ALU.mult), r=[eqm, sc], w=[eqm])
                    A('dve', lambda e: e.tensor_reduce(out=wsum[:], in_=eqm[:], axis=AX.X, op=ALU.add), r=[eqm], w=[wsum])
                    A('dve', lambda e: e.reciprocal(out=wsum[:], in_=wsum[:]), r=[wsum], w=[wsum])
                    A('dve', lambda e: e.tensor_scalar(out=Wr[:, i, 0:NEXP], in0=eqm[:], scalar1=wsum[:, 0:1], scalar2=2.5, op0=ALU.mult, op1=ALU.mult), r=[eqm, wsum], w=[Wr])
                    if dbg:
                        A('sp', lambda e: e.dma_start(out=d_W[i * 128:(i + 1) * 128, :], in_=Wr[:, i, :]), r=[Wr], dma=True)

                for i in range(0 if stop == 'pre' else n_main):
                    main_tile(i)
                S.barrier()
            if stop in ('pre', 'main'):
                raise _Stop()

            with ExitStack() as p2:
                acc = sbuf(p2, "acc", [128, NT, D], F32)
                wgu = [sbuf(p2, "wgu%d" % i, [128, KC, 512], BF16) for i in range(2)]
                wd = [sbuf(p2, "wd%d" % i, [128, 2, D], BF16) for i in range(2)]
                sg = [[sbuf(p2, "sg%d_%d" % (i, j), [128, 512], F32) for j in range(2)] for i in range(2)]
                hm = [[sbuf(p2, "hm%d_%d" % (i, j), [128, 512], BF16) for j in range(2)] for i in range(2)]
                xf = sbuf(p2, "xf", [128, D], F32)
                xl = sbuf(p2, "xl", [128, D], F32)
                sq2 = sbuf(p2, "sq2", [128, D], F32)
                ss2 = sbuf(p2, "ss2", [128, 4], F32)
                ot = sbuf(p2, "ot", [128, D], F32)
                A('pool', lambda e: e.memset(acc[:], 0.0), w=[acc])
                accr = [Res("acc%d" % t) for t in range(NT)]
                for t in range(NT):
                    accr[t].w = acc.r.w

                wst = [sbuf(p2, "wst%d" % i, [128, 2048], F32) for i in range(2)]
                ldc = [0]

                def load_w(ei):
                    s_ = ei % 2
                    if ei < NEXP:
                        g_src, u_src, d_src = w_gate_d[ei], w_up_d[ei], w_down_d[ei]
                    else:
                        g_src, u_src, d_src = ws_gate_d, ws_up_d, ws_down_d
                    for which, src in enumerate((g_src, u_src, d_src)):
                        st = wst[ldc[0] % 2]
                        ldc[0] += 1
                        if which < 2:
                            A('sp', lambda e, st=st, src=src: e.dma_start(out=st[:].rearrange("p (c f) -> p c f", c=KC), in_=src.rearrange("(c p) f -> p c f", p=128)), w=[st], dma=True)
                            A('pool', lambda e, st=st, which=which, s_=s_: e.tensor_copy(out=wgu[s_][:, :, which * 256:(which + 1) * 256], in_=st[:].rearrange("p (c f) -> p c f", c=KC)), r=[st], w=[wgu[s_]])
                        else:
                            A('sp', lambda e, st=st, src=src: e.dma_start(out=st[:].rearrange("p (c f) -> p c f", c=2), in_=src.rearrange("(c p) f -> p c f", p=128)), w=[st], dma=True)
                            A('pool', lambda e, st=st, s_=s_: e.tensor_copy(out=wd[s_][:].rearrange("p c f -> p (c f)"), in_=st[:]), r=[st], w=[wd[s_]])

                elist = list(range(n_exp_decl)) + [NEXP]
                load_w(elist[0])
                step = 0
                for idx, ei in enumerate(elist):
                    if idx + 1 < len(elist):
                        load_w(elist[idx + 1])
                    s = ei % 2
                    for tg in range(4):
                        par = step % 2
                        step += 1
                        tsl = slice(tg * 512, (tg + 1) * 512)
                        for half in range(2):
                            for k in range(KC):
                                A('pe', lambda e, k=k, half=half, s=s, tsl=tsl: e.matmul(PB[half][:], lhsT=wgu[s][:, k, half * 128:(half + 1) * 128], rhs=h2T[:, k, tsl], start=(k == 0), stop=(k == KC - 1)), r=[wgu[s], h2T], w=[PB[half]])
                            for k in range(KC):
                                A('pe', lambda e, k=k, half=half, s=s, tsl=tsl: e.matmul(PB[2 + half][:], lhsT=wgu[s][:, k, 256 + half * 128:256 + (half + 1) * 128], rhs=h2T[:, k, tsl], start=(k == 0), stop=(k == KC - 1)), r=[wgu[s], h2T], w=[PB[2 + half]])
                        for half in range(2):
                            A('act', lambda e, half=half, par=par: e.activation(out=sg[par][half][:], in_=PB[half][:], func=AF.Silu), r=[PB[half]], w=[sg[par][half]])
                            A('dve', lambda e, half=half, par=par: e.scalar_tensor_tensor(out=hm[par][half][:], in0=PB[2 + half][:], scalar=1.0, in1=sg[par][half][:], op0=ALU.mult, op1=ALU.mult), r=[PB[2 + half], sg[par][half]], w=[hm[par][half]])
                        for pr in range(2):
                            tl = (pr * 2, pr * 2 + 1)
                            for half in range(2):
                                for t4 in tl:
                                    yb = (PB[4], PB[5]) if t4 % 2 == 0 else (PB[6], PB[7])
                                    for dh in range(2):
                                        A('pe', lambda e, dh=dh, half=half, t4=t4, par=par, s=s, yb=yb: e.matmul(yb[dh][:], lhsT=hm[par][half][:, t4 * 128:(t4 + 1) * 128], rhs=wd[s][:, half, dh * 512:(dh + 1) * 512], start=(half == 0), stop=(half == 1)), r=[hm[par][half], wd[s]], w=[yb[dh]])
                                    if half == 1:
                                        tile = tg * 4 + t4
                                        for dh in range(2):
                                            S.op('dve', lambda e, dh=dh, tile=tile, ei=ei, yb=yb: e.scalar_tensor_tensor(out=acc[:, tile, dh * 512:(dh + 1) * 512], in0=yb[dh][:], scalar=Wr[:, tile, ei:ei + 1], in1=acc[:, tile, dh * 512:(dh + 1) * 512], op0=ALU.mult, op1=ALU.add), reads=[yb[dh].r, Wr.r, accr[tile]], writes=[accr[tile]])
                for t in range(NT):
                    A('sp', lambda e, t=t: e.dma_start(out=xl[:], in_=x1s_d[t * 128:(t + 1) * 128, :]), w=[xl], dma=True)
                    S.op('dve', lambda e, t=t: e.tensor_tensor(out=xf[:], in0=acc[:, t, :], in1=gt2_b[:], op=ALU.mult), reads=[accr[t], gt2_b.r], writes=[xf.r])
                    A('dve', lambda e: e.tensor_tensor(out=xf[:], in0=xf[:], in1=xl[:], op=ALU.add), r=[xf, xl], w=[xf])
                    A('act', lambda e: e.activation(out=sq2[:], in_=xf[:], func=AF.Square, accum_out=ss2[:, 0:1]), r=[xf], w=[sq2, ss2])
                    A('act', lambda e: e.activation(out=ss2[:, 1:2], in_=ss2[:, 0:1], func=AF.Sqrt, scale=1.0 / D, bias=EPS), r=[ss2], w=[ss2])
                    A('dve', lambda e: e.reciprocal(out=ss2[:, 2:3], in_=ss2[:, 1:2]), r=[ss2], w=[ss2])
                    A('dve', lambda e: e.scalar_tensor_tensor(out=ot[:], in0=xf[:], scalar=ss2[:, 2:3], in1=gfin_b[:], op0=ALU.mult, op1=ALU.mult), r=[xf, ss2, gfin_b], w=[ot])
                    A('sp', lambda e, t=t: e.dma_start(out=out_d[t * 128:(t + 1) * 128, :], in_=ot[:]), r=[ot], dma=True)
        except _Stop:
            dummy = sbuf(es, 'dummy', [128, D], F32)
            A('dve', lambda e: e.memset(dummy[:], 1.0), w=[dummy])
            A('sp', lambda e: e.dma_start(out=out_d[0:128, :], in_=dummy[:]), r=[dummy], dma=True)
        S.wait_all('sp')
        stats = S.emit()
    return nc, stats


_CACHE = {}


def make_in_maps(x, c, positions, w_ada, b_ada, g_attn, w_in, w_gk_up, b_gk, g_gla_out, sinks, w_out,
                 g_ffn, w_router, router_bias, w_gate, w_up, w_down, ws_gate, ws_up, ws_down, g_final):
    f = np.float32
    x = np.asarray(x, f)
    shared = {
        "w_ada": np.ascontiguousarray(np.asarray(w_ada, f)[0]),
        "b_ada_col": np.ascontiguousarray(np.asarray(b_ada, f)[0].reshape(48, 128).T),
        "b_ada": np.ascontiguousarray(np.asarray(b_ada, f)[0].reshape(1, -1)),
        "g_attn_col": np.ascontiguousarray(np.asarray(g_attn, f)[0].reshape(8, 128).T),
        "g_ffn_col": np.ascontiguousarray(np.asarray(g_ffn, f)[0].reshape(8, 128).T),
        "g_final": np.ascontiguousarray(np.asarray(g_final, f).reshape(1, -1)),
        "w_in": np.ascontiguousarray(np.asarray(w_in, f)[0]),
        "w_gk_up": np.ascontiguousarray(np.asarray(w_gk_up, f)[0]),
        "b_gk": np.ascontiguousarray(np.asarray(b_gk, f)[0].reshape(1, -1)),
        "g_gla": np.ascontiguousarray(np.asarray(g_gla_out, f)[0].reshape(1, -1)),
        "sinks": np.ascontiguousarray(np.asarray(sinks, f)[0].reshape(1, -1)),
        "w_out": np.ascontiguousarray(np.asarray(w_out, f)[0]),
        "w_router": np.ascontiguousarray(np.asarray(w_router, f)[0]),
        "router_bias": np.ascontiguousarray(np.asarray(router_bias, f)[0].reshape(1, -1)),
        "w_gate": np.ascontiguousarray(np.asarray(w_gate, f)[0]),
        "w_up": np.ascontiguousarray(np.asarray(w_up, f)[0]),
        "w_down": np.ascontiguousarray(np.asarray(w_down, f)[0]),
        "ws_gate": np.ascontiguousarray(np.asarray(ws_gate, f)[0]),
        "ws_up": np.ascontiguousarray(np.asarray(ws_up, f)[0]),
        "ws_down": np.ascontiguousarray(np.asarray(ws_down, f)[0]),
    }
    positions = np.asarray(positions, np.int32)
    c = np.asarray(c, f)
    qi = np.arange(128)[:, None]
    kj = np.arange(128)[None, :]
    band = np.concatenate([np.where(kj > qi, 0.0, NEG), np.where(kj <= qi, 0.0, NEG)], axis=1).astype(f)
    in_maps = []
    for core in range(N_CORES):
        b, s = core // 4, core % 4
        t0 = s * TOK
        m = dict(shared)
        m["x_main"] = np.ascontiguousarray(x[b, t0:t0 + TOK])
        xp = np.zeros((NPRE * T, D), f)
        va = np.zeros((NPRE * T,), f)
        if s > 0:
            xp[NPRE * T - t0:] = x[b, :t0]
            va[NPRE * T - t0:] = 1.0
        m["x_pre"] = xp
        m["valid"] = np.ascontiguousarray(va.reshape(NPRE, T).T)
        pos = np.zeros((T, NT + 1), np.int32)
        pos[:, 1:] = positions[b, t0:t0 + TOK].reshape(NT, T).T
        if s > 0:
            pos[:, 0] = positions[b, t0 - T:t0]
        m["pos"] = pos
        mk = band.copy()
        if s == 0:
            mk[:, 0:128] = NEG
        m["mask0"] = mk
        m["c_col"] = np.ascontiguousarray(c[b].reshape(8, 128).T)
        in_maps.append(m)
    return in_maps


def kernel(**inputs):
    if "nc" not in _CACHE:
        _CACHE["nc"] = build_program()[0]
    nc = _CACHE["nc"]
    in_maps = make_in_maps(**inputs)
    res = run_bass_kernel_spmd(nc, in_maps, core_ids=list(range(N_CORES)))
    out = np.zeros((2, 4 * TOK, D), np.float32)
    for core in range(N_CORES):
        b, s = core // 4, core % 4
        out[b, s * TOK:(s + 1) * TOK] = res.results[core]["out"]
    return out
```

```python
import math
from contextlib import ExitStack

import numpy as np
import concourse.bass as bass
import concourse.mybir as mybir
from concourse.bass_utils import run_bass_kernel_spmd

F32 = mybir.dt.float32
BF16 = mybir.dt.bfloat16
I32 = mybir.dt.int32
AF = mybir.ActivationFunctionType
ALU = mybir.AluOpType
AX = mybir.AxisListType

ENGS = ['pe', 'act', 'dve', 'pool', 'sp']
NDMA = 24

N_CORES = 8
D = 1024
KC = 8
T = 128
NT = 16
NPRE = 48
TOK = NT * T
NEXP = 256
EPS = 1e-6
NEG = -30000.0

C_GQ, C_GK, C_GV, C_GLOW, C_GR, C_SQ, C_SK, C_SV = 0, 256, 512, 1024, 1040, 1552, 2064, 2192
IN_W = 2320


class Res:
    __slots__ = ('name', 'w', 'r', 'excl')

    def __init__(self, name, excl=False):
        self.name = name
        self.w = None
        self.r = {}
        self.excl = excl


class Sched:
    def __init__(self, nc, es):
        self.nc = nc
        self.ops = {e: [] for e in ENGS}
        self.esem = {e: es.enter_context(nc.semaphore('s_' + e)) for e in ENGS}
        self.dsem = [es.enter_context(nc.semaphore('d%d' % i)) for i in range(NDMA)]
        self.dcount = [0] * NDMA
        self.dnext = 0
        self.waited = {e: {} for e in ENGS}

    def _need(self, eng, ev, waits, is_dma):
        if ev is None:
            return
        if ev[0] == 'e':
            _, E, i = ev
            if E == eng and eng == 'pe' and not is_dma:
                return
            if E == eng and eng == 'sp':
                return
            key = ('e', E)
            if self.waited[eng].get(key, -1) >= i:
                return
            self.waited[eng][key] = i
            self.ops[E][i]['signal'] = True
            waits.append(ev)
        else:
            _, k, v = ev
            key = ('d', k)
            if self.waited[eng].get(key, 0) >= v:
                return
            self.waited[eng][key] = v
            waits.append(ev)

    def op(self, eng, fn, reads=(), writes=(), dma=False):
        waits = []
        for R in reads:
            self._need(eng, R.w, waits, dma)
            if R.excl:
                for ev in R.r.values():
                    if ev[0] == 'e' and ev[1] == eng:
                        continue
                    self._need(eng, ev, waits, dma)
        for R in writes:
            self._need(eng, R.w, waits, dma)
            for ev in R.r.values():
                self._need(eng, ev, waits, dma)
        rec = {'fn': fn, 'waits': waits, 'signal': False, 'dma': None}
        idx = len(self.ops[eng])
        if dma:
            k = self.dnext
            self.dnext = (self.dnext + 1) % NDMA
            if self.dcount[k] > 0:
                self._need(eng, ('d', k, self.dcount[k]), waits, dma)
            self.dcount[k] += 16
            rec['dma'] = k
            ev = ('d', k, self.dcount[k])
        else:
            ev = ('e', eng, idx)
        self.ops[eng].append(rec)
        for R in reads:
            R.r[ev[:2]] = ev
        for R in writes:
            R.w = ev
            R.r = {}
        return ev

    def wait_all(self, eng):
        waits = []
        for E in ENGS:
            if E != eng and E != 'sp' and self.ops[E]:
                i = len(self.ops[E]) - 1
                while i >= 0 and (self.ops[E][i]['fn'] is None or self.ops[E][i]['dma'] is not None):
                    i -= 1
                if i >= 0:
                    self._need(eng, ('e', E, i), waits, False)
        for k in range(NDMA):
            if self.dcount[k] > 0:
                self._need(eng, ('d', k, self.dcount[k]), waits, False)
        self.ops[eng].append({'fn': None, 'waits': waits, 'signal': False, 'dma': None})

    def barrier(self):
        for E in ENGS:
            self.wait_all(E)

    def emit(self):
        nc = self.nc
        pref = {}
        for E in ENGS:
            c = 0
            arr = []
            for o in self.ops[E]:
                if o['signal']:
                    c += 1
                arr.append(c)
            pref[E] = arr
        stats = {}

        def run(E, eng):
            nw = 0
            for o in self.ops[E]:
                for ev in o['waits']:
                    nw += 1
                    if ev[0] == 'e':
                        eng.wait_ge(self.esem[ev[1]], pref[ev[1]][ev[2]])
                    else:
                        eng.wait_ge(self.dsem[ev[1]], ev[2])
                if o['fn'] is None:
                    continue
                if o['fn'] == 'nop':
                    inst = eng.nop()
                else:
                    inst = o['fn'](eng)
                if o['signal']:
                    inst.then_inc(self.esem[E], 1)
                if o['dma'] is not None:
                    inst.then_inc(self.dsem[o['dma']], 16)
            stats[E] = (len(self.ops[E]), nw)

        with nc.Block() as block:
            @block.tensor
            def _(eng):
                run('pe', eng)

            @block.scalar
            def _(eng):
                run('act', eng)

            @block.vector
            def _(eng):
                run('dve', eng)

            @block.gpsimd
            def _(eng):
                run('pool', eng)

            @block.sync
            def _(eng):
                run('sp', eng)
        return stats


class Buf:
    __slots__ = ('t', 'r')

    def __init__(self, t, r):
        self.t = t
        self.r = r

    def __getitem__(self, k):
        return self.t[k]


class _Stop(Exception):
    pass


def build_program(n_exp_decl=NEXP, dbg=False, n_pre=NPRE, n_main=NT, stop=None):
    nc = bass.Bass("TRN2", target_bir_lowering=False)

    def din(name, shape, dt=F32):
        return nc.dram_tensor(name, list(shape), dt, kind="ExternalInput").ap()

    x_main = din("x_main", [TOK, D])
    x_pre = din("x_pre", [NPRE * T, D])
    valid_d = din("valid", [T, NPRE])
    pos_d = din("pos", [T, NT + 1], I32)
    mask0_d = din("mask0", [T, 256])
    c_col_d = din("c_col", [T, KC])
    w_ada_d = din("w_ada", [D, 6 * D])
    b_ada_col_d = din("b_ada_col", [T, 48])
    b_ada_d = din("b_ada", [1, 6 * D])
    g_attn_col_d = din("g_attn_col", [T, KC])
    g_ffn_col_d = din("g_ffn_col", [T, KC])
    g_final_d = din("g_final", [1, D])
    w_in_d = din("w_in", [D, IN_W])
    w_gk_up_d = din("w_gk_up", [16, 256])
    b_gk_d = din("b_gk", [1, 256])
    g_gla_d = din("g_gla", [1, 128])
    sinks_d = din("sinks", [1, 8])
    w_out_d = din("w_out", [D, D])
    w_router_d = din("w_router", [D, NEXP])
    rbias_d = din("router_bias", [1, NEXP])
    w_gate_d = din("w_gate", [n_exp_decl, D, 256])
    w_up_d = din("w_up", [n_exp_decl, D, 256])
    w_down_d = din("w_down", [n_exp_decl, 256, D])
    ws_gate_d = din("ws_gate", [D, 256])
    ws_up_d = din("ws_up", [D, 256])
    ws_down_d = din("ws_down", [256, D])
    out_d = nc.dram_tensor("out", [TOK, D], F32, kind="ExternalOutput").ap()
    x1s_d = nc.dram_tensor("x1s", [TOK, D], F32, kind="Internal").ap()
    if dbg:
        d_x1 = nc.dram_tensor("d_x1", [TOK, D], F32, kind="ExternalOutput").ap()
        d_mix = nc.dram_tensor("d_mix", [TOK, D], F32, kind="ExternalOutput").ap()
        d_W = nc.dram_tensor("d_W", [TOK, NEXP + 1], F32, kind="ExternalOutput").ap()

    inv_freq = [float(np.float32(np.power(np.float32(500000.0), np.float32(-(2 * i) / 16.0)))) for i in range(8)]

    with ExitStack() as es:
        S = Sched(nc, es)

        def sbuf(stack, name, shape, dt):
            return Buf(stack.enter_context(nc.sbuf_tensor("sb_" + name, list(shape), dt)), Res(name))

        def A(eng, fn, r=(), w=(), dma=False):
            S.op(eng, fn, reads=[b.r for b in r], writes=[b.r for b in w], dma=dma)

        PB = [Buf(es.enter_context(nc.psum_tensor("pb%d" % i, [128, 512], F32)), Res("pb%d" % i, excl=True)) for i in range(8)]

        h2T = sbuf(es, "h2T", [128, KC, TOK], BF16)
        Wr = sbuf(es, "Wr", [128, NT, NEXP + 1], F32)
        gt1_b = sbuf(es, "gt1_b", [128, D], F32)
        gt2_b = sbuf(es, "gt2_b", [128, D], F32)
        gfin_b = sbuf(es, "gfin_b", [128, D], F32)
        identf = sbuf(es, "identf", [128, 128], F32)
        ident = sbuf(es, "ident", [128, 128], BF16)
        triT = sbuf(es, "triT", [128, 128], F32)
        ustr = sbuf(es, "ustr", [128, 128], F32)
        ones_f = sbuf(es, "ones_f", [128, 128], F32)
        maskb = sbuf(es, "maskb", [128, 256], F32)
        mask0 = sbuf(es, "mask0", [128, 256], F32)
        modcol = sbuf(es, "modcol", [128, 32], F32)
        s1c = sbuf(es, "s1c", [128, KC], F32)
        s2c = sbuf(es, "s2c", [128, KC], F32)
        rbias_b = sbuf(es, "rbias_b", [128, NEXP], F32)
        ggla_b = sbuf(es, "ggla_b", [128, 512], F32)
        sink_b = sbuf(es, "sink_b", [128, 8], F32)
        cosA = sbuf(es, "cosA", [128, NT + 1, 8], F32)
        sinA = sbuf(es, "sinA", [128, NT + 1, 8], F32)
        valid = sbuf(es, "valid", [128, NPRE], F32)

        try:
            A('pool', lambda e: e.memset(ones_f[:], 1.0), w=[ones_f])
            A('pool', lambda e: e.memset(identf[:], 1.0), w=[identf])
            A('pool', lambda e: e.affine_select(out=identf[:], in_=identf[:], pattern=[[1, 128]], compare_op=ALU.is_equal, fill=0.0, base=0, channel_multiplier=-1), r=[identf], w=[identf])
            A('dve', lambda e: e.tensor_copy(out=ident[:], in_=identf[:]), r=[identf], w=[ident])
            A('pool', lambda e: e.affine_select(out=triT[:], in_=ones_f[:], pattern=[[1, 128]], compare_op=ALU.is_ge, fill=0.0, base=0, channel_multiplier=-1), r=[ones_f], w=[triT])
            A('pool', lambda e: e.affine_select(out=ustr[:], in_=ones_f[:], pattern=[[-1, 128]], compare_op=ALU.is_ge, fill=0.0, base=-1, channel_multiplier=1), r=[ones_f], w=[ustr])
            A('pool', lambda e: e.memset(maskb[:], 0.0), w=[maskb])
            A('pool', lambda e: e.affine_select(out=maskb[:, 0:128], in_=maskb[:, 0:128], pattern=[[1, 128]], compare_op=ALU.is_ge, fill=NEG, base=-1, channel_multiplier=-1), r=[maskb], w=[maskb])
            A('pool', lambda e: e.affine_select(out=maskb[:, 128:256], in_=maskb[:, 128:256], pattern=[[-1, 128]], compare_op=ALU.is_ge, fill=NEG, base=0, channel_multiplier=1), r=[maskb], w=[maskb])
            A('sp', lambda e: e.dma_start(out=mask0[:], in_=mask0_d), w=[mask0], dma=True)
            A('sp', lambda e: e.dma_start(out=valid[:], in_=valid_d), w=[valid], dma=True)
            A('sp', lambda e: e.dma_start(out=rbias_b[:], in_=rbias_d.partition_broadcast(128)), w=[rbias_b], dma=True)
            A('sp', lambda e: e.dma_start(out=sink_b[:], in_=sinks_d.partition_broadcast(128)), w=[sink_b], dma=True)
            A('sp', lambda e: e.dma_start(out=gfin_b[:], in_=g_final_d.partition_broadcast(128)), w=[gfin_b], dma=True)
            for h in range(4):
                A('sp', lambda e, h=h: e.dma_start(out=ggla_b[:, h * 128:(h + 1) * 128], in_=g_gla_d.partition_broadcast(128)), w=[ggla_b], dma=True)
            A('sp', lambda e: e.dma_start(out=gt1_b[:], in_=b_ada_d[:, 2 * D:3 * D].partition_broadcast(128)), w=[gt1_b], dma=True)
            A('sp', lambda e: e.dma_start(out=gt2_b[:], in_=b_ada_d[:, 5 * D:6 * D].partition_broadcast(128)), w=[gt2_b], dma=True)
            A('pool', lambda e: e.memset(Wr[:, :, NEXP:NEXP + 1], 1.0), w=[Wr])
            hmk = sbuf(es, 'hmk', [128, 4], F32)
            A('pool', lambda e: e.memset(hmk[:], 0.0), w=[hmk])
            A('pool', lambda e: e.memset(hmk[0:64, 0:1], 1.0), r=[hmk], w=[hmk])
            A('pool', lambda e: e.memset(hmk[64:128, 1:2], 1.0), r=[hmk], w=[hmk])
            A('pool', lambda e: e.memset(hmk[0:64, 2:3], 0.125), r=[hmk], w=[hmk])
            A('pool', lambda e: e.memset(hmk[64:128, 3:4], 0.125), r=[hmk], w=[hmk])

            if stop == 'const':
                raise _Stop()
            with ExitStack() as p0:
                posi = sbuf(p0, "posi", [128, NT + 1], I32)
                posf = sbuf(p0, "posf", [128, NT + 1], F32)
                ang = sbuf(p0, "ang", [128, 2, NT + 1, 8], F32)
                kf = sbuf(p0, "kf", [128, 2, NT + 1, 8], F32)
                ki = sbuf(p0, "ki", [128, 2, NT + 1, 8], I32)
                A('sp', lambda e: e.dma_start(out=posi[:], in_=pos_d), w=[posi], dma=True)
                A('dve', lambda e: e.tensor_copy(out=posf[:], in_=posi[:]), r=[posi], w=[posf])
                for f in range(8):
                    A('dve', lambda e, f=f: e.tensor_scalar(out=ang[:, 0, :, f], in0=posf[:], scalar1=inv_freq[f], scalar2=None, op0=ALU.mult), r=[posf], w=[ang])
                A('dve', lambda e: e.tensor_scalar(out=ang[:, 1], in0=ang[:, 0], scalar1=math.pi / 2, scalar2=None, op0=ALU.add), r=[ang], w=[ang])
                A('dve', lambda e: e.tensor_scalar(out=kf[:], in0=ang[:], scalar1=1.0 / (2 * math.pi), scalar2=None, op0=ALU.mult), r=[ang], w=[kf])
                A('dve', lambda e: e.tensor_copy(out=ki[:], in_=kf[:]), r=[kf], w=[ki])
                A('dve', lambda e: e.tensor_copy(out=kf[:], in_=ki[:]), r=[ki], w=[kf])
                A('dve', lambda e: e.scalar_tensor_tensor(out=ang[:], in0=kf[:], scalar=-2 * math.pi, in1=ang[:], op0=ALU.mult, op1=ALU.add), r=[kf, ang], w=[ang])
                A('dve', lambda e: e.tensor_single_scalar(out=kf[:], in_=ang[:], scalar=math.pi, op=ALU.is_gt), r=[ang], w=[kf])
                A('dve', lambda e: e.scalar_tensor_tensor(out=ang[:], in0=kf[:], scalar=-2 * math.pi, in1=ang[:], op0=ALU.mult, op1=ALU.add), r=[kf, ang], w=[ang])
                A('dve', lambda e: e.tensor_single_scalar(out=kf[:], in_=ang[:], scalar=-math.pi, op=ALU.is_lt), r=[ang], w=[kf])
                A('dve', lambda e: e.scalar_tensor_tensor(out=ang[:], in0=kf[:], scalar=2 * math.pi, in1=ang[:], op0=ALU.mult, op1=ALU.add), r=[kf, ang], w=[ang])
                A('act', lambda e: e.activation(out=sinA[:], in_=ang[:, 0], func=AF.Sin), r=[ang], w=[sinA])
                A('act', lambda e: e.activation(out=cosA[:], in_=ang[:, 1], func=AF.Sin), r=[ang], w=[cosA])

                if stop == 'rope':
                    raise _Stop()
                c_col = sbuf(p0, "c_col", [128, KC], F32)
                cact = sbuf(p0, "cact", [128, KC], F32)
                crep = sbuf(p0, "crep", [128, KC, 128], F32)
                bcol = sbuf(p0, "bcol", [128, 48], F32)
                gac = sbuf(p0, "gac", [128, KC], F32)
                gfc = sbuf(p0, "gfc", [128, KC], F32)
                wa = [sbuf(p0, "wa%d" % i, [128, KC, 512], F32) for i in range(2)]
                A('sp', lambda e: e.dma_start(out=c_col[:], in_=c_col_d), w=[c_col], dma=True)
                A('sp', lambda e: e.dma_start(out=bcol[:], in_=b_ada_col_d), w=[bcol], dma=True)
                A('sp', lambda e: e.dma_start(out=gac[:], in_=g_attn_col_d), w=[gac], dma=True)
                A('sp', lambda e: e.dma_start(out=gfc[:], in_=g_ffn_col_d), w=[gfc], dma=True)
                A('act', lambda e: e.activation(out=cact[:], in_=c_col[:], func=AF.Silu), r=[c_col], w=[cact])
                for k in range(KC):
                    A('dve', lambda e, k=k: e.tensor_scalar(out=crep[:, k, :], in0=ones_f[:], scalar1=cact[:, k:k + 1], scalar2=None, op0=ALU.mult), r=[ones_f, cact], w=[crep])
                colq = 0
                for j in range(12):
                    wb = wa[j % 2]
                    A('sp', lambda e, j=j, wb=wb: e.dma_start(out=wb[:], in_=w_ada_d[:, j * 512:(j + 1) * 512].rearrange("(c p) f -> p c f", p=128)), w=[wb], dma=True)
                    if j in (4, 5, 10, 11):
                        dst = gt1_b if j in (4, 5) else gt2_b
                        half = j % 2
                        pbk = PB[1 + half]
                        for k in range(KC):
                            A('pe', lambda e, k=k, wb=wb, pbk=pbk: e.matmul(pbk[:], lhsT=crep[:, k, :], rhs=wb[:, k, :], start=(k == 0), stop=(k == KC - 1)), r=[crep, wb], w=[pbk])
                        A('dve', lambda e, dst=dst, half=half, pbk=pbk: e.tensor_tensor(out=dst[:, half * 512:(half + 1) * 512], in0=pbk[:], in1=dst[:, half * 512:(half + 1) * 512], op=ALU.add), r=[pbk, dst], w=[dst])
                    else:
                        for q in range(4):
                            for k in range(KC):
                                A('pe', lambda e, k=k, q=q, wb=wb, cq=colq: e.matmul(PB[0][:, cq:cq + 1], lhsT=wb[:, k, q * 128:(q + 1) * 128], rhs=cact[:, k:k + 1], start=(k == 0), stop=(k == KC - 1)), r=[cact, wb], w=[PB[0]])
                            colq += 1
                A('dve', lambda e: e.tensor_tensor(out=modcol[:, 0:16], in0=PB[0][:, 0:16], in1=bcol[:, 0:16], op=ALU.add), r=[PB[0], bcol], w=[modcol])
                A('dve', lambda e: e.tensor_tensor(out=modcol[:, 16:32], in0=PB[0][:, 16:32], in1=bcol[:, 24:40], op=ALU.add), r=[PB[0], bcol], w=[modcol])
                A('dve', lambda e: e.scalar_tensor_tensor(out=s1c[:], in0=modcol[:, 8:16], scalar=1.0, in1=gac[:], op0=ALU.add, op1=ALU.mult), r=[modcol, gac], w=[s1c])
                A('dve', lambda e: e.scalar_tensor_tensor(out=s2c[:], in0=modcol[:, 24:32], scalar=1.0, in1=gfc[:], op0=ALU.add, op1=ALU.mult), r=[modcol, gfc], w=[s2c])
                S.barrier()
            if stop == 'ada':
                raise _Stop()

            with ExitStack() as p1:
                w_in = sbuf(p1, "w_in", [128, KC, IN_W], BF16)
                w_out = sbuf(p1, "w_out", [128, KC, D], BF16)
                w_rt = sbuf(p1, "w_rt", [128, KC, NEXP], BF16)
                wgk = sbuf(p1, "wgk", [16, 256], F32)
                bgk = sbuf(p1, "bgk", [1, 256], F32)
                stg = sbuf(p1, "stg", [128, IN_W], F32)
                for k in range(KC):
                    A('sp', lambda e, k=k: e.dma_start(out=stg[:], in_=w_in_d[k * 128:(k + 1) * 128, :]), w=[stg], dma=True)
                    A('pool', lambda e, k=k: e.tensor_copy(out=w_in[:, k, :], in_=stg[:]), r=[stg], w=[w_in])
                for k in range(KC):
                    A('sp', lambda e, k=k: e.dma_start(out=stg[:, 0:D], in_=w_out_d[k * 128:(k + 1) * 128, :]), w=[stg], dma=True)
                    A('pool', lambda e, k=k: e.tensor_copy(out=w_out[:, k, :], in_=stg[:, 0:D]), r=[stg], w=[w_out])
                A('sp', lambda e: e.dma_start(out=stg[:, 0:KC * NEXP].rearrange("p (c f) -> p c f", c=KC), in_=w_router_d.rearrange("(c p) f -> p c f", p=128)), w=[stg], dma=True)
                A('pool', lambda e: e.tensor_copy(out=w_rt[:].rearrange("p c f -> p (c f)"), in_=stg[:, 0:KC * NEXP]), r=[stg], w=[w_rt])
                A('sp', lambda e: e.dma_start(out=wgk[:], in_=w_gk_up_d), w=[wgk], dma=True)
                A('sp', lambda e: e.dma_start(out=bgk[:], in_=b_gk_d), w=[bgk], dma=True)

                xt = sbuf(p1, "xt", [128, D], F32)
                sqj = sbuf(p1, "sqj", [128, D], F32)
                ss = sbuf(p1, "ss", [128, 4], F32)
                xs = sbuf(p1, "xs", [128, D], BF16)
                hT = sbuf(p1, "hT", [128, KC, 128], BF16)
                glowT = sbuf(p1, "glowT", [16, 128], F32)
                ab = sbuf(p1, "ab", [128, 256], F32)
                ex = sbuf(p1, "ex", [128, 256], F32)
                la = sbuf(p1, "la", [128, 256], F32)
                E1 = sbuf(p1, "E1", [128, 256], F32)
                E2 = sbuf(p1, "E2", [128, 256], F32)
                E3 = sbuf(p1, "E3", [128, 256], F32)
                decc = sbuf(p1, "decc", [128, 2], F32)
                qdT = sbuf(p1, "qdT", [128, 4, 128], BF16)
                kdT = sbuf(p1, "kdT", [128, 4, 128], BF16)
                kdec = sbuf(p1, "kdec", [128, 256], BF16)
                v_bf = sbuf(p1, "v_bf", [128, 512], BF16)
                attnT = sbuf(p1, "attnT", [128, 4, 128], BF16)
                Sst = sbuf(p1, "Sst", [128, 2, 128], F32)
                S_bf = sbuf(p1, "S_bf", [128, 2, 128], BF16)
                ssg = sbuf(p1, "ssg", [128, 4], F32)
                rstdg = sbuf(p1, "rstdg", [128, 4], F32)
                gsil = sbuf(p1, "gsil", [128, 512], F32)
                mix = sbuf(p1, "mix", [128, D], BF16)
                mixT = sbuf(p1, "mixT", [128, KC, 128], BF16)
                q_r = sbuf(p1, "q_r", [128, 4, 2, 64], BF16)
                rt1 = sbuf(p1, "rt1", [128, 4, 2, 8], F32)
                rt2 = sbuf(p1, "rt2", [128, 4, 2, 8], F32)
                kv_r = [sbuf(p1, "kv_r%d" % i, [128, 256], BF16) for i in range(2)]
                kT = [sbuf(p1, "kT%d" % i, [128, 128], BF16) for i in range(2)]
                qT = sbuf(p1, "qT", [128, 2, 4, 128], BF16)
                sm = sbuf(p1, "sm", [128, 4, 256], F32)
                rmax = sbuf(p1, "rmax", [128, 8], F32)
                negm = sbuf(p1, "negm", [128, 8], F32)
                rsum = sbuf(p1, "rsum", [128, 8], F32)
                esk = sbuf(p1, "esk", [128, 8], F32)
                pb = sbuf(p1, "pb", [128, 4, 256], BF16)
                pT = sbuf(p1, "pT", [128, 8, 128], BF16)
                x1 = sbuf(p1, "x1", [128, D], F32)
                tmpm = sbuf(p1, "tmpm", [128, D], F32)
                sc = sbuf(p1, "sc", [128, NEXP], F32)
                sel = sbuf(p1, "sel", [128, NEXP], F32)
                sel2 = sbuf(p1, "sel2", [128, NEXP], F32)
                eqm = sbuf(p1, "eqm", [128, NEXP], F32)
                m1 = sbuf(p1, "m1", [128, 8], F32)
                m2 = sbuf(p1, "m2", [128, 8], F32)
                gs = sbuf(p1, "gs", [128, 8], F32)
                top8 = sbuf(p1, "top8", [128, 8], F32)
                pen = sbuf(p1, "pen", [128, 8], F32)
                wsum = sbuf(p1, "wsum", [128, 1], F32)

                A('dve', lambda e: e.memset(Sst[:], 0.0), w=[Sst])
                A('dve', lambda e: e.memset(S_bf[:], 0.0), w=[S_bf])

                def bfv(pbuf):
                    return pbuf[:].bitcast(BF16)

                def front(xsrc, scol, bcol_, hT_dst):
                    A('sp', lambda e: e.dma_start(out=xt[:], in_=xsrc), w=[xt], dma=True)
                    front_from_sbuf(xt, scol, bcol_, hT_dst)

                def front_from_sbuf(xsb, scol, bcol_, hT_dst, hT_buf=None):
                    hb = hT if hT_buf is None else hT_buf
                    A('act', lambda e: e.activation(out=sqj[:], in_=xsb[:], func=AF.Square, accum_out=ss[:, 0:1]), r=[xsb], w=[sqj, ss])
                    A('act', lambda e: e.activation(out=ss[:, 1:2], in_=ss[:, 0:1], func=AF.Sqrt, scale=1.0 / D, bias=EPS), r=[ss], w=[ss])
                    A('dve', lambda e: e.reciprocal(out=ss[:, 2:3], in_=ss[:, 1:2]), r=[ss], w=[ss])
                    A('dve', lambda e: e.tensor_scalar(out=xs[:], in0=xsb[:], scalar1=ss[:, 2:3], scalar2=None, op0=ALU.mult), r=[xsb, ss], w=[xs])
                    pv = bfv(PB[0])
                    for c in range(KC):
                        A('pe', lambda e, c=c: e.transpose(out=pv[:, c * 128:(c + 1) * 128], in_=xs[:, c * 128:(c + 1) * 128], identity=ident[:]), r=[xs, ident], w=[PB[0]])
                    for c in range(KC):
                        A('act', lambda e, c=c: e.activation(out=hT_dst(c), in_=pv[:, c * 128:(c + 1) * 128], func=AF.Identity, scale=scol[:, c:c + 1], bias=bcol_[:, c:c + 1]), r=[PB[0], scol, bcol_], w=[hb])

                def proj_tok(pbk, col0, col1, c_lo, c_hi):
                    n = c_hi - c_lo
                    for k in range(KC):
                        A('pe', lambda e, k=k: e.matmul(pbk[:, col0:col0 + n], lhsT=hT[:, k, :], rhs=w_in[:, k, c_lo:c_hi], start=(k == 0), stop=(k == KC - 1)), r=[hT, w_in], w=[pbk])

                def proj_feat(pbk, col0, c_lo, m):
                    for k in range(KC):
                        A('pe', lambda e, k=k: e.matmul(pbk[0:m, col0:col0 + 128], lhsT=w_in[:, k, c_lo:c_lo + m], rhs=hT[:, k, :], start=(k == 0), stop=(k == KC - 1)), r=[hT, w_in], w=[pbk])

                def gate_logs():
                    A('dve', lambda e: e.tensor_copy(out=glowT[:], in_=PB[6][0:16, 0:128]), r=[PB[6]], w=[glowT])
                    A('pe', lambda e: e.matmul(PB[6][:, 128:384], lhsT=glowT[:], rhs=wgk[:], start=True, stop=False), r=[glowT, wgk], w=[PB[6]])
                    A('pe', lambda e: e.matmul(PB[6][:, 128:384], lhsT=ones_f[0:1, :], rhs=bgk[:], start=False, stop=True), r=[ones_f, bgk], w=[PB[6]])
                    pre = PB[6][:, 128:384]
                    A('act', lambda e: e.activation(out=ab[:], in_=pre, func=AF.Abs), r=[PB[6]], w=[ab])
                    A('act', lambda e: e.activation(out=ex[:], in_=ab[:], func=AF.Exp, scale=-1.0), r=[ab], w=[ex])
                    A('act', lambda e: e.activation(out=ex[:], in_=ex[:], func=AF.Ln, bias=1.0), r=[ex], w=[ex])
                    A('dve', lambda e: e.scalar_tensor_tensor(out=la[:], in0=pre, scalar=0.0, in1=ex[:], op0=ALU.min, op1=ALU.subtract), r=[PB[6], ex], w=[la])
                    A('dve', lambda e: e.tensor_scalar(out=la[:], in0=la[:], scalar1=1.0 / 16.0, scalar2=None, op0=ALU.mult), r=[la], w=[la])

                def state_update():
                    for p in range(2):
                        A('pe', lambda e, p=p: e.matmul(PB[7][:, p * 256:(p + 1) * 256], lhsT=kdec[:, p * 128:(p + 1) * 128], rhs=v_bf[:, p * 256:(p + 1) * 256], start=True, stop=True), r=[kdec, v_bf], w=[PB[7]])
                    for p in range(2):
                        for w_ in range(2):
                            rs = slice(w_ * 64, (w_ + 1) * 64)
                            A('dve', lambda e, p=p, w_=w_, rs=rs: e.scalar_tensor_tensor(out=Sst[rs, p, :], in0=Sst[rs, p, :], scalar=decc[rs, p:p + 1], in1=PB[7][rs, p * 256 + w_ * 128:p * 256 + (w_ + 1) * 128], op0=ALU.mult, op1=ALU.add), r=[Sst, decc, PB[7]], w=[Sst])
                    A('act', lambda e: e.copy(out=S_bf[:], in_=Sst[:]), r=[Sst], w=[S_bf])

                def prefix_tile(t, last):
                    front(x_pre[t * 128:(t + 1) * 128, :], s1c, modcol_sh1, lambda c: hT[:, c, :])
                    proj_tok(PB[1], 0, 512, C_GV, C_GV + 512)
                    if last:
                        proj_tok(PB[4], 0, 256, C_SK, C_SK + 256)
                        proj_tok(PB[4], 256, 512, C_GK, C_GK + 256)
                    else:
                        proj_tok(PB[4], 256, 512, C_GK, C_GK + 256)
                    proj_feat(PB[6], 0, C_GLOW, 16)
                    gate_logs()
                    A('pe', lambda e: e.matmul(PB[7][:, 256:512], lhsT=ustr[:], rhs=la[:], start=True, stop=True), r=[ustr, la], w=[PB[7]])
                    for p in range(2):
                        A('pe', lambda e, p=p: e.matmul(PB[7][:, p:p + 1], lhsT=la[:, p * 128:(p + 1) * 128], rhs=ones_f[:, 0:1], start=True, stop=True), r=[la, ones_f], w=[PB[7]])
                    A('act', lambda e: e.activation(out=E3[:], in_=PB[7][:, 256:512], func=AF.Exp), r=[PB[7]], w=[E3])
                    A('act', lambda e: e.activation(out=decc[:], in_=PB[7][:, 0:2], func=AF.Exp), r=[PB[7]], w=[decc])
                    A('dve', lambda e: e.scalar_tensor_tensor(out=kdec[:], in0=PB[4][:, 256:512], scalar=valid[:, t:t + 1], in1=E3[:], op0=ALU.mult, op1=ALU.mult), r=[PB[4], valid, E3], w=[kdec])
                    A('act', lambda e: e.copy(out=v_bf[:], in_=PB[1][:]), r=[PB[1]], w=[v_bf])
                    if last:
                        rope_kv(0, 0)
                    state_update()

                def rope_kv(slot, tcol):
                    kvb = kv_r[slot]
                    A('dve', lambda e: e.tensor_copy(out=kvb[:], in_=PB[4][:, 0:256]), r=[PB[4]], w=[kvb])
                    kv3 = PB[4][:, 0:128].rearrange("p (h d) -> p h d", h=2)
                    ko3 = kvb[:, 0:128].rearrange("p (h d) -> p h d", h=2)
                    cb = cosA[:, tcol, :].unsqueeze(1).to_broadcast([128, 2, 8])
                    sb_ = sinA[:, tcol, :].unsqueeze(1).to_broadcast([128, 2, 8])
                    r1 = rt1[:, 0, :, :]
                    r2 = rt2[:, 0, :, :]
                    A('dve', lambda e: e.tensor_tensor(out=r1, in0=kv3[:, :, 0:8], in1=cb, op=ALU.mult), r=[PB[4], cosA], w=[rt1])
                    A('dve', lambda e: e.tensor_tensor(out=r2, in0=kv3[:, :, 8:16], in1=sb_, op=ALU.mult), r=[PB[4], sinA], w=[rt2])
                    A('dve', lambda e: e.tensor_tensor(out=ko3[:, :, 0:8], in0=r1, in1=r2, op=ALU.subtract), r=[rt1, rt2], w=[kvb])
                    A('dve', lambda e: e.tensor_tensor(out=r1, in0=kv3[:, :, 8:16], in1=cb, op=ALU.mult), r=[PB[4], cosA], w=[rt1])
                    A('dve', lambda e: e.tensor_tensor(out=r2, in0=kv3[:, :, 0:8], in1=sb_, op=ALU.mult), r=[PB[4], sinA], w=[rt2])
                    A('dve', lambda e: e.tensor_tensor(out=ko3[:, :, 8:16], in0=r1, in1=r2, op=ALU.add), r=[rt1, rt2], w=[kvb])
                    pv = bfv(PB[0])
                    A('pe', lambda e: e.transpose(out=pv[:, 0:128], in_=kvb[:, 0:128], identity=ident[:]), r=[kvb, ident], w=[PB[0]])
                    A('act', lambda e: e.copy(out=kT[slot][:], in_=pv[:, 0:128]), r=[PB[0]], w=[kT[slot]])

                modcol_sh1 = Buf(modcol.t[:, 0:8], modcol.r)
                modcol_sh2 = Buf(modcol.t[:, 16:24], modcol.r)

                for t in range(NPRE - n_pre, NPRE):
                    prefix_tile(t, t == NPRE - 1)

                def main_tile(i):
                    cur = (i + 1) % 2
                    prv = i % 2
                    tcol = i + 1
                    front(x_main[i * 128:(i + 1) * 128, :], s1c, modcol_sh1, lambda c: hT[:, c, :])
                    proj_tok(PB[1], 0, 512, C_GV, C_GV + 512)
                    proj_tok(PB[2], 0, 512, C_GR, C_GR + 512)
                    proj_tok(PB[3], 0, 512, C_SQ, C_SQ + 512)
                    proj_tok(PB[4], 0, 256, C_SK, C_SK + 256)
                    proj_tok(PB[4], 256, 512, C_GK, C_GK + 256)
                    for p in range(2):
                        proj_feat(PB[5], p * 128, C_GQ + p * 128, 128)
                    for p in range(2):
                        proj_feat(PB[5], 256 + p * 128, C_GK + p * 128, 128)
                    proj_feat(PB[6], 0, C_GLOW, 16)
                    gate_logs()
                    for p in range(2):
                        A('pe', lambda e, p=p: e.matmul(PB[7][:, p * 128:(p + 1) * 128], lhsT=la[:, p * 128:(p + 1) * 128], rhs=triT[:], start=True, stop=True), r=[la, triT], w=[PB[7]])
                    A('pe', lambda e: e.matmul(PB[7][:, 256:512], lhsT=ustr[:], rhs=la[:], start=True, stop=True), r=[ustr, la], w=[PB[7]])
                    A('act', lambda e: e.activation(out=E1[:], in_=PB[7][:, 0:256], func=AF.Exp), r=[PB[7]], w=[E1])
                    A('act', lambda e: e.activation(out=E2[:], in_=PB[7][:, 0:256], func=AF.Exp, scale=-1.0), r=[PB[7]], w=[E2])
                    A('act', lambda e: e.activation(out=E3[:], in_=PB[7][:, 256:512], func=AF.Exp), r=[PB[7]], w=[E3])
                    A('dve', lambda e: e.tensor_copy(out=decc[:, 0:1], in_=E1[:, 127:128]), r=[E1], w=[decc])
                    A('dve', lambda e: e.tensor_copy(out=decc[:, 1:2], in_=E1[:, 255:256]), r=[E1], w=[decc])
                    for h_ in range(4):
                        p_, w__ = h_ // 2, h_ % 2
                        A('dve', lambda e, h_=h_, p_=p_, w__=w__: e.scalar_tensor_tensor(out=qdT[:, h_, :], in0=PB[5][:, p_ * 128:(p_ + 1) * 128], scalar=hmk[:, 2 + w__:3 + w__], in1=E1[:, p_ * 128:(p_ + 1) * 128], op0=ALU.mult, op1=ALU.mult), r=[PB[5], E1, hmk], w=[qdT])
                    for h_ in range(4):
                        p_, w__ = h_ // 2, h_ % 2
                        A('dve', lambda e, h_=h_, p_=p_, w__=w__: e.scalar_tensor_tensor(out=kdT[:, h_, :], in0=PB[5][:, 256 + p_ * 128:256 + (p_ + 1) * 128], scalar=hmk[:, w__:w__ + 1], in1=E2[:, p_ * 128:(p_ + 1) * 128], op0=ALU.mult, op1=ALU.mult), r=[PB[5], E2, hmk], w=[kdT])
                    A('dve', lambda e: e.scalar_tensor_tensor(out=kdec[:], in0=PB[4][:, 256:512], scalar=1.0, in1=E3[:], op0=ALU.mult, op1=ALU.mult), r=[PB[4], E3], w=[kdec])
                    A('act', lambda e: e.copy(out=v_bf[:], in_=PB[1][:]), r=[PB[1]], w=[v_bf])
                    for h in range(4):
                        p, w_ = h // 2, h % 2
                        rs = slice(w_ * 64, (w_ + 1) * 64)
                        A('pe', lambda e, h=h, p=p, rs=rs: e.matmul(PB[5][:, h * 128:(h + 1) * 128], lhsT=kdT[:, h, :], rhs=qdT[:, h, :], start=True, stop=True), r=[kdT, qdT], w=[PB[5]])
                    for h in range(4):
                        A('dve', lambda e, h=h: e.scalar_tensor_tensor(out=attnT[:, h, :], in0=PB[5][:, h * 128:(h + 1) * 128], scalar=1.0, in1=triT[:], op0=ALU.mult, op1=ALU.mult), r=[PB[5], triT], w=[attnT])
                    for h in range(4):
                        p, w_ = h // 2, h % 2
                        rs = slice(w_ * 64, (w_ + 1) * 64)
                        A('pe', lambda e, h=h: e.matmul(PB[1][:, h * 128:(h + 1) * 128], lhsT=attnT[:, h, :], rhs=v_bf[:, h * 128:(h + 1) * 128], start=True, stop=False), r=[attnT, v_bf], w=[PB[1]])
                        A('pe', lambda e, h=h, p=p, rs=rs: e.matmul(PB[1][:, h * 128:(h + 1) * 128], lhsT=qdT[:, h, :], rhs=S_bf[:, p, :], start=False, stop=True), r=[qdT, S_bf], w=[PB[1]])
                    state_update()
                    for h in range(4):
                        A('act', lambda e, h=h: e.activation(out=sqj[:, h * 128:(h + 1) * 128], in_=PB[1][:, h * 128:(h + 1) * 128], func=AF.Square, accum_out=ssg[:, h:h + 1]), r=[PB[1]], w=[sqj, ssg])
                    A('act', lambda e: e.activation(out=ssg[:], in_=ssg[:], func=AF.Sqrt, scale=1.0 / 128, bias=EPS), r=[ssg], w=[ssg])
                    A('dve', lambda e: e.reciprocal(out=rstdg[:], in_=ssg[:]), r=[ssg], w=[rstdg])
                    A('act', lambda e: e.activation(out=gsil[:], in_=PB[2][:], func=AF.Silu), r=[PB[2]], w=[gsil])
                    A('dve', lambda e: e.tensor_tensor(out=gsil[:], in0=gsil[:], in1=ggla_b[:], op=ALU.mult), r=[gsil, ggla_b], w=[gsil])
                    for h in range(4):
                        A('dve', lambda e, h=h: e.scalar_tensor_tensor(out=mix[:, h * 128:(h + 1) * 128], in0=PB[1][:, h * 128:(h + 1) * 128], scalar=rstdg[:, h:h + 1], in1=gsil[:, h * 128:(h + 1) * 128], op0=ALU.mult, op1=ALU.mult), r=[PB[1], rstdg, gsil], w=[mix])
                    rope_kv(cur, tcol)
                    q4 = PB[3][:].rearrange("p (w a d) -> p a w d", w=2, a=4)
                    A('act', lambda e: e.copy(out=q_r[:], in_=q4), r=[PB[3]], w=[q_r])
                    cb = cosA[:, tcol, :].unsqueeze(1).unsqueeze(1).to_broadcast([128, 4, 2, 8])
                    sb_ = sinA[:, tcol, :].unsqueeze(1).unsqueeze(1).to_broadcast([128, 4, 2, 8])
                    A('dve', lambda e: e.tensor_tensor(out=rt1[:], in0=q4[:, :, :, 0:8], in1=cb, op=ALU.mult), r=[PB[3], cosA], w=[rt1])
                    A('dve', lambda e: e.tensor_tensor(out=rt2[:], in0=q4[:, :, :, 8:16], in1=sb_, op=ALU.mult), r=[PB[3], sinA], w=[rt2])
                    A('dve', lambda e: e.tensor_tensor(out=q_r[:, :, :, 0:8], in0=rt1[:], in1=rt2[:], op=ALU.subtract), r=[rt1, rt2], w=[q_r])
                    A('dve', lambda e: e.tensor_tensor(out=rt1[:], in0=q4[:, :, :, 8:16], in1=cb, op=ALU.mult), r=[PB[3], cosA], w=[rt1])
                    A('dve', lambda e: e.tensor_tensor(out=rt2[:], in0=q4[:, :, :, 0:8], in1=sb_, op=ALU.mult), r=[PB[3], sinA], w=[rt2])
                    A('dve', lambda e: e.tensor_tensor(out=q_r[:, :, :, 8:16], in0=rt1[:], in1=rt2[:], op=ALU.add), r=[rt1, rt2], w=[q_r])
                    pv0 = bfv(PB[0])
                    for a in range(4):
                        A('pe', lambda e, a=a: e.transpose(out=pv0[:, a * 128:(a + 1) * 128], in_=q_r[:, a, :, :].rearrange("p w d -> p (w d)"), identity=ident[:]), r=[q_r, ident], w=[PB[0]])
                    for w__ in range(2):
                        A('dve', lambda e, w__=w__: e.tensor_scalar(out=qT[:, w__, :, :].rearrange("p a t -> p (a t)"), in0=pv0[:, 0:512], scalar1=hmk[:, w__:w__ + 1], scalar2=None, op0=ALU.mult), r=[PB[0], hmk], w=[qT])
                    mk = mask0 if i == 0 else maskb
                    for g in range(2):
                        banks = (PB[3], PB[4]) if g == 0 else (PB[2], PB[6])
                        for ai in range(2):
                            a = g * 2 + ai
                            bk = banks[ai]
                            for w_ in range(2):
                                rs = slice(w_ * 64, (w_ + 1) * 64)
                                A('pe', lambda e, a=a, w_=w_, rs=rs, bk=bk: e.matmul(bk[:, w_ * 256:w_ * 256 + 128], lhsT=qT[:, w_, a, :], rhs=kT[prv][:, :], start=True, stop=True), r=[qT, kT[prv]], w=[bk])
                                A('pe', lambda e, a=a, w_=w_, rs=rs, bk=bk: e.matmul(bk[:, w_ * 256 + 128:w_ * 256 + 256], lhsT=qT[:, w_, a, :], rhs=kT[cur][:, :], start=True, stop=True), r=[qT, kT[cur]], w=[bk])
                        for ai in range(2):
                            bk = banks[ai]
                            for w_ in range(2):
                                j = ai * 2 + w_
                                A('dve', lambda e, j=j, w_=w_, bk=bk, mk=mk: e.scalar_tensor_tensor(out=sm[:, j, :], in0=bk[:, w_ * 256:(w_ + 1) * 256], scalar=0.125, in1=mk[:], op0=ALU.mult, op1=ALU.add), r=[bk, mk], w=[sm])
                        gc = slice(g * 4, g * 4 + 4)
                        A('dve', lambda e, gc=gc: e.tensor_reduce(out=rmax[:, gc], in_=sm[:], axis=AX.X, op=ALU.max), r=[sm], w=[rmax])
                        for ai in range(2):
                            for w_ in range(2):
                                j = g * 4 + ai * 2 + w_
                                hq = w_ * 4 + g * 2 + ai
                                A('dve', lambda e, j=j, hq=hq: e.tensor_tensor(out=rmax[:, j:j + 1], in0=rmax[:, j:j + 1], in1=sink_b[:, hq:hq + 1], op=ALU.max), r=[rmax, sink_b], w=[rmax])
                                A('dve', lambda e, j=j, hq=hq: e.tensor_tensor(out=esk[:, j:j + 1], in0=sink_b[:, hq:hq + 1], in1=rmax[:, j:j + 1], op=ALU.subtract), r=[rmax, sink_b], w=[esk])
                        A('dve', lambda e, gc=gc: e.tensor_scalar(out=negm[:, gc], in0=rmax[:, gc], scalar1=-1.0, scalar2=None, op0=ALU.mult), r=[rmax], w=[negm])
                        for j in range(4):
                            A('act', lambda e, j=j, g=g: e.activation(out=pb[:, j, :], in_=sm[:, j, :], func=AF.Exp, bias=negm[:, g * 4 + j:g * 4 + j + 1], accum_out=rsum[:, g * 4 + j:g * 4 + j + 1]), r=[sm, negm], w=[pb, rsum])
                        A('act', lambda e, gc=gc: e.activation(out=esk[:, gc], in_=esk[:, gc], func=AF.Exp), r=[esk], w=[esk])
                        A('dve', lambda e, gc=gc: e.tensor_tensor(out=rsum[:, gc], in0=rsum[:, gc], in1=esk[:, gc], op=ALU.add), r=[rsum, esk], w=[rsum])
                        A('dve', lambda e, gc=gc: e.reciprocal(out=rsum[:, gc], in_=rsum[:, gc]), r=[rsum], w=[rsum])
                        tb = PB[5] if g == 0 else PB[7]
                        tv = bfv(tb)
                        for j in range(4):
                            for blk in range(2):
                                A('pe', lambda e, j=j, blk=blk, tv=tv: e.transpose(out=tv[:, (j * 2 + blk) * 128:(j * 2 + blk + 1) * 128], in_=pb[:, j, blk * 128:(blk + 1) * 128], identity=ident[:]), r=[pb, ident], w=[tb])
                        A('act', lambda e, tv=tv: e.copy(out=pT[:].rearrange("p a t -> p (a t)"), in_=tv), r=[tb], w=[pT])
                        for ai in range(2):
                            for w_ in range(2):
                                j = ai * 2 + w_
                                hq = w_ * 4 + g * 2 + ai
                                A('pe', lambda e, j=j, hq=hq, w_=w_: e.matmul(PB[1][:, hq * 64:(hq + 1) * 64], lhsT=pT[:, j * 2, :], rhs=kv_r[prv][:, 128 + w_ * 64:128 + (w_ + 1) * 64], start=True, stop=False), r=[pT, kv_r[prv]], w=[PB[1]])
                                A('pe', lambda e, j=j, hq=hq, w_=w_: e.matmul(PB[1][:, hq * 64:(hq + 1) * 64], lhsT=pT[:, j * 2 + 1, :], rhs=kv_r[cur][:, 128 + w_ * 64:128 + (w_ + 1) * 64], start=False, stop=True), r=[pT, kv_r[cur]], w=[PB[1]])
                        for ai in range(2):
                            for w_ in range(2):
                                j = g * 4 + ai * 2 + w_
                                hq = w_ * 4 + g * 2 + ai
                                A('dve', lambda e, j=j, hq=hq: e.tensor_scalar(out=mix[:, 512 + hq * 64:512 + (hq + 1) * 64], in0=PB[1][:, hq * 64:(hq + 1) * 64], scalar1=rsum[:, j:j + 1], scalar2=None, op0=ALU.mult), r=[PB[1], rsum], w=[mix])
                    pv = bfv(PB[0])
                    for c in range(KC):
                        A('pe', lambda e, c=c: e.transpose(out=pv[:, c * 128:(c + 1) * 128], in_=mix[:, c * 128:(c + 1) * 128], identity=ident[:]), r=[mix, ident], w=[PB[0]])
                    A('act', lambda e: e.copy(out=mixT[:].rearrange("p c t -> p (c t)"), in_=pv), r=[PB[0]], w=[mixT])
                    for dh in range(2):
                        bk = PB[4] if dh == 0 else PB[6]
                        for k in range(KC):
                            A('pe', lambda e, k=k, dh=dh, bk=bk: e.matmul(bk[:], lhsT=mixT[:, k, :], rhs=w_out[:, k, dh * 512:(dh + 1) * 512], start=(k == 0), stop=(k == KC - 1)), r=[mixT, w_out], w=[bk])
                        A('dve', lambda e, dh=dh, bk=bk: e.tensor_tensor(out=tmpm[:, dh * 512:(dh + 1) * 512], in0=bk[:], in1=gt1_b[:, dh * 512:(dh + 1) * 512], op=ALU.mult), r=[bk, gt1_b], w=[tmpm])
                    A('dve', lambda e: e.tensor_tensor(out=x1[:], in0=tmpm[:], in1=xt[:], op=ALU.add), r=[tmpm, xt], w=[x1])
                    A('sp', lambda e: e.dma_start(out=x1s_d[i * 128:(i + 1) * 128, :], in_=x1[:]), r=[x1], dma=True)
                    if dbg:
                        A('sp', lambda e: e.dma_start(out=d_x1[i * 128:(i + 1) * 128, :], in_=x1[:]), r=[x1], dma=True)
                        A('dve', lambda e: e.tensor_copy(out=tmpm[:], in_=mix[:]), r=[mix], w=[tmpm])
                        A('sp', lambda e: e.dma_start(out=d_mix[i * 128:(i + 1) * 128, :], in_=tmpm[:]), r=[tmpm], dma=True)
                    front_from_sbuf(x1, s2c, modcol_sh2, lambda c: h2T[:, c, i * 128:(i + 1) * 128], hT_buf=h2T)
                    for k in range(KC):
                        A('pe', lambda e, k=k: e.matmul(PB[5][:, 0:NEXP], lhsT=h2T[:, k, i * 128:(i + 1) * 128], rhs=w_rt[:, k, :], start=(k == 0), stop=(k == KC - 1)), r=[h2T, w_rt], w=[PB[5]])
                    A('act', lambda e: e.activation(out=sc[:], in_=PB[5][:, 0:NEXP], func=AF.Sigmoid), r=[PB[5]], w=[sc])
                    A('dve', lambda e: e.tensor_tensor(out=sel[:], in0=sc[:], in1=rbias_b[:], op=ALU.add), r=[sc, rbias_b], w=[sel])
                    sel3 = sel[:].rearrange("p (g k) -> p g k", g=8)
                    A('dve', lambda e: e.tensor_reduce(out=m1[:], in_=sel3, axis=AX.X, op=ALU.max), r=[sel], w=[m1])
                    A('dve', lambda e: e.tensor_tensor(out=eqm[:].rearrange("p (g k) -> p g k", g=8), in0=sel3, in1=m1[:].unsqueeze(2).to_broadcast([128, 8, 32]), op=ALU.is_equal), r=[sel, m1], w=[eqm])
                    A('dve', lambda e: e.scalar_tensor_tensor(out=sel2[:], in0=eqm[:], scalar=-1e30, in1=sel[:], op0=ALU.mult, op1=ALU.add), r=[eqm, sel], w=[sel2])
                    A('dve', lambda e: e.tensor_reduce(out=m2[:], in_=sel2[:].rearrange("p (g k) -> p g k", g=8), axis=AX.X, op=ALU.max), r=[sel2], w=[m2])
                    A('dve', lambda e: e.tensor_tensor(out=gs[:], in0=m1[:], in1=m2[:], op=ALU.add), r=[m1, m2], w=[gs])
                    A('dve', lambda e: e.max(out=top8[:], in_=gs[:]), r=[gs], w=[top8])
                    A('dve', lambda e: e.tensor_scalar(out=pen[:], in0=gs[:], scalar1=top8[:, 3:4], scalar2=1e30, op0=ALU.is_lt, op1=ALU.mult), r=[gs, top8], w=[pen])
                    A('dve', lambda e: e.tensor_tensor(out=sel2[:].rearrange("p (g k) -> p g k", g=8), in0=sel3, in1=pen[:].unsqueeze(2).to_broadcast([128, 8, 32]), op=ALU.subtract), r=[sel, pen], w=[sel2])
                    A('dve', lambda e: e.max(out=top8[:], in_=sel2[:]), r=[sel2], w=[top8])
                    A('dve', lambda e: e.tensor_scalar(out=eqm[:], in0=sel2[:], scalar1=top8[:, 7:8], scalar2=None, op0=ALU.is_ge), r=[sel2, top8], w=[eqm])
                    A('dve', lambda e: e.tensor_tensor(out=eqm[:], in0=eqm[:], in1=sc[:], op=ALU.mult), r=[eqm, sc], w=[eqm])
                    A('dve', lambda e: e.tensor_reduce(out=wsum[:], in_=eqm[:], axis=AX.X, op=ALU.add), r=[eqm], w=[wsum])
                    A('dve', lambda e: e.reciprocal(out=wsum[:], in_=wsum[:]), r=[wsum], w=[wsum])
                    A('dve', lambda e: e.tensor_scalar(out=Wr[:, i, 0:NEXP], in0=eqm[:], scalar1=wsum[:, 0:1], scalar2=2.5, op0=ALU.mult, op1=ALU.mult), r=[eqm, wsum], w=[Wr])
                    if dbg:
                        A('sp', lambda e: e.dma_start(out=d_W[i * 128:(i + 1) * 128, :], in_=Wr[:, i, :]), r=[Wr], dma=True)

                for i in range(0 if stop == 'pre' else n_main):
                    main_tile(i)
                S.barrier()
            if stop in ('pre', 'main'):
                raise _Stop()

            with ExitStack() as p2:
                acc = sbuf(p2, "acc", [128, NT, D], F32)
                wgu = [sbuf(p2, "wgu%d" % i, [128, KC, 512], BF16) for i in range(2)]
                wd = [sbuf(p2, "wd%d" % i, [128, 2, D], BF16) for i in range(2)]
                sg = [sbuf(p2, "sg%d" % i, [128, 2, 512], F32) for i in range(2)]
                hm = [sbuf(p2, "hm%d" % i, [128, 2, 512], BF16) for i in range(2)]
                xf = sbuf(p2, "xf", [128, D], F32)
                xl = sbuf(p2, "xl", [128, D], F32)
                sq2 = sbuf(p2, "sq2", [128, D], F32)
                ss2 = sbuf(p2, "ss2", [128, 4], F32)
                ot = sbuf(p2, "ot", [128, D], F32)
                A('pool', lambda e: e.memset(acc[:], 0.0), w=[acc])
                accr = [Res("acc%d" % t) for t in range(NT)]
                for t in range(NT):
                    accr[t].w = acc.r.w

                wst = [sbuf(p2, "wst%d" % i, [128, 2048], F32) for i in range(2)]
                ldc = [0]

                def load_w(ei):
                    s_ = ei % 2
                    if ei < NEXP:
                        g_src, u_src, d_src = w_gate_d[ei], w_up_d[ei], w_down_d[ei]
                    else:
                        g_src, u_src, d_src = ws_gate_d, ws_up_d, ws_down_d
                    for which, src in enumerate((g_src, u_src, d_src)):
                        st = wst[ldc[0] % 2]
                        ldc[0] += 1
                        if which < 2:
                            A('sp', lambda e, st=st, src=src: e.dma_start(out=st[:].rearrange("p (c f) -> p c f", c=KC), in_=src.rearrange("(c p) f -> p c f", p=128)), w=[st], dma=True)
                            A('pool', lambda e, st=st, which=which, s_=s_: e.tensor_copy(out=wgu[s_][:, :, which * 256:(which + 1) * 256], in_=st[:].rearrange("p (c f) -> p c f", c=KC)), r=[st], w=[wgu[s_]])
                        else:
                            A('sp', lambda e, st=st, src=src: e.dma_start(out=st[:].rearrange("p (c f) -> p c f", c=2), in_=src.rearrange("(c p) f -> p c f", p=128)), w=[st], dma=True)
                            A('pool', lambda e, st=st, s_=s_: e.tensor_copy(out=wd[s_][:].rearrange("p c f -> p (c f)"), in_=st[:]), r=[st], w=[wd[s_]])

                elist = list(range(n_exp_decl)) + [NEXP]
                load_w(elist[0])
                step = 0
                for idx, ei in enumerate(elist):
                    if idx + 1 < len(elist):
                        load_w(elist[idx + 1])
                    s = ei % 2
                    for tg in range(4):
                        par = step % 2
                        step += 1
                        tsl = slice(tg * 512, (tg + 1) * 512)
                        for half in range(2):
                            for k in range(KC):
                                A('pe', lambda e, k=k, half=half, s=s, tsl=tsl: e.matmul(PB[half][:], lhsT=wgu[s][:, k, half * 128:(half + 1) * 128], rhs=h2T[:, k, tsl], start=(k == 0), stop=(k == KC - 1)), r=[wgu[s], h2T], w=[PB[half]])
                            for k in range(KC):
                                A('pe', lambda e, k=k, half=half, s=s, tsl=tsl: e.matmul(PB[2 + half][:], lhsT=wgu[s][:, k, 256 + half * 128:256 + (half + 1) * 128], rhs=h2T[:, k, tsl], start=(k == 0), stop=(k == KC - 1)), r=[wgu[s], h2T], w=[PB[2 + half]])
                        for half in range(2):
                            A('act', lambda e, half=half, par=par: e.activation(out=sg[par][:, half, :], in_=PB[half][:], func=AF.Silu), r=[PB[half]], w=[sg[par]])
                            A('dve', lambda e, half=half, par=par: e.scalar_tensor_tensor(out=hm[par][:, half, :], in0=PB[2 + half][:], scalar=1.0, in1=sg[par][:, half, :], op0=ALU.mult, op1=ALU.mult), r=[PB[2 + half], sg[par]], w=[hm[par]])
                        for t4 in range(4):
                            tile = tg * 4 + t4
                            yb = (PB[4], PB[5]) if t4 % 2 == 0 else (PB[6], PB[7])
                            for dh in range(2):
                                for half in range(2):
                                    A('pe', lambda e, dh=dh, half=half, t4=t4, par=par, s=s, yb=yb: e.matmul(yb[dh][:], lhsT=hm[par][:, half, t4 * 128:(t4 + 1) * 128], rhs=wd[s][:, half, dh * 512:(dh + 1) * 512], start=(half == 0), stop=(half == 1)), r=[hm[par], wd[s]], w=[yb[dh]])
                            for dh in range(2):
                                S.op('dve', lambda e, dh=dh, tile=tile, ei=ei, yb=yb: e.scalar_tensor_tensor(out=acc[:, tile, dh * 512:(dh + 1) * 512], in0=yb[dh][:], scalar=Wr[:, tile, ei:ei + 1], in1=acc[:, tile, dh * 512:(dh + 1) * 512], op0=ALU.mult, op1=ALU.add), reads=[yb[dh].r, Wr.r, accr[tile]], writes=[accr[tile]])
                for t in range(NT):
                    A('sp', lambda e, t=t: e.dma_start(out=xl[:], in_=x1s_d[t * 128:(t + 1) * 128, :]), w=[xl], dma=True)
                    S.op('dve', lambda e, t=t: e.tensor_tensor(out=xf[:], in0=acc[:, t, :], in1=gt2_b[:], op=ALU.mult), reads=[accr[t], gt2_b.r], writes=[xf.r])
                    A('dve', lambda e: e.tensor_tensor(out=xf[:], in0=xf[:], in1=xl[:], op=ALU.add), r=[xf, xl], w=[xf])
                    A('act', lambda e: e.activation(out=sq2[:], in_=xf[:], func=AF.Square, accum_out=ss2[:, 0:1]), r=[xf], w=[sq2, ss2])
                    A('act', lambda e: e.activation(out=ss2[:, 1:2], in_=ss2[:, 0:1], func=AF.Sqrt, scale=1.0 / D, bias=EPS), r=[ss2], w=[ss2])
                    A('dve', lambda e: e.reciprocal(out=ss2[:, 2:3], in_=ss2[:, 1:2]), r=[ss2], w=[ss2])
                    A('dve', lambda e: e.scalar_tensor_tensor(out=ot[:], in0=xf[:], scalar=ss2[:, 2:3], in1=gfin_b[:], op0=ALU.mult, op1=ALU.mult), r=[xf, ss2, gfin_b], w=[ot])
                    A('sp', lambda e, t=t: e.dma_start(out=out_d[t * 128:(t + 1) * 128, :], in_=ot[:]), r=[ot], dma=True)
        except _Stop:
            dummy = sbuf(es, 'dummy', [128, D], F32)
            A('dve', lambda e: e.memset(dummy[:], 1.0), w=[dummy])
            A('sp', lambda e: e.dma_start(out=out_d[0:128, :], in_=dummy[:]), r=[dummy], dma=True)
        S.wait_all('sp')
        stats = S.emit()
    return nc, stats


_CACHE = {}


def make_in_maps(x, c, positions, w_ada, b_ada, g_attn, w_in, w_gk_up, b_gk, g_gla_out, sinks, w_out,
                 g_ffn, w_router, router_bias, w_gate, w_up, w_down, ws_gate, ws_up, ws_down, g_final):
    f = np.float32
    x = np.asarray(x, f)
    shared = {
        "w_ada": np.ascontiguousarray(np.asarray(w_ada, f)[0]),
        "b_ada_col": np.ascontiguousarray(np.asarray(b_ada, f)[0].reshape(48, 128).T),
        "b_ada": np.ascontiguousarray(np.asarray(b_ada, f)[0].reshape(1, -1)),
        "g_attn_col": np.ascontiguousarray(np.asarray(g_attn, f)[0].reshape(8, 128).T),
        "g_ffn_col": np.ascontiguousarray(np.asarray(g_ffn, f)[0].reshape(8, 128).T),
        "g_final": np.ascontiguousarray(np.asarray(g_final, f).reshape(1, -1)),
        "w_in": np.ascontiguousarray(np.asarray(w_in, f)[0]),
        "w_gk_up": np.ascontiguousarray(np.asarray(w_gk_up, f)[0]),
        "b_gk": np.ascontiguousarray(np.asarray(b_gk, f)[0].reshape(1, -1)),
        "g_gla": np.ascontiguousarray(np.asarray(g_gla_out, f)[0].reshape(1, -1)),
        "sinks": np.ascontiguousarray(np.asarray(sinks, f)[0].reshape(1, -1)),
        "w_out": np.ascontiguousarray(np.asarray(w_out, f)[0]),
        "w_router": np.ascontiguousarray(np.asarray(w_router, f)[0]),
        "router_bias": np.ascontiguousarray(np.asarray(router_bias, f)[0].reshape(1, -1)),
        "w_gate": np.ascontiguousarray(np.asarray(w_gate, f)[0]),
        "w_up": np.ascontiguousarray(np.asarray(w_up, f)[0]),
        "w_down": np.ascontiguousarray(np.asarray(w_down, f)[0]),
        "ws_gate": np.ascontiguousarray(np.asarray(ws_gate, f)[0]),
        "ws_up": np.ascontiguousarray(np.asarray(ws_up, f)[0]),
        "ws_down": np.ascontiguousarray(np.asarray(ws_down, f)[0]),
    }
    positions = np.asarray(positions, np.int32)
    c = np.asarray(c, f)
    qi = np.arange(128)[:, None]
    kj = np.arange(128)[None, :]
    band = np.concatenate([np.where(kj > qi, 0.0, NEG), np.where(kj <= qi, 0.0, NEG)], axis=1).astype(f)
    in_maps = []
    for core in range(N_CORES):
        b, s = core // 4, core % 4
        t0 = s * TOK
        m = dict(shared)
        m["x_main"] = np.ascontiguousarray(x[b, t0:t0 + TOK])
        xp = np.zeros((NPRE * T, D), f)
        va = np.zeros((NPRE * T,), f)
        if s > 0:
            xp[NPRE * T - t0:] = x[b, :t0]
            va[NPRE * T - t0:] = 1.0
        m["x_pre"] = xp
        m["valid"] = np.ascontiguousarray(va.reshape(NPRE, T).T)
        pos = np.zeros((T, NT + 1), np.int32)
        pos[:, 1:] = positions[b, t0:t0 + TOK].reshape(NT, T).T
        if s > 0:
            pos[:, 0] = positions[b, t0 - T:t0]
        m["pos"] = pos
        mk = band.copy()
        if s == 0:
            mk[:, 0:128] = NEG
        m["mask0"] = mk
        m["c_col"] = np.ascontiguousarray(c[b].reshape(8, 128).T)
        in_maps.append(m)
    return in_maps


def kernel(**inputs):
    if "nc" not in _CACHE:
        _CACHE["nc"] = build_program()[0]
    nc = _CACHE["nc"]
    in_maps = make_in_maps(**inputs)
    res = run_bass_kernel_spmd(nc, in_maps, core_ids=list(range(N_CORES)))
    out = np.zeros((2, 4 * TOK, D), np.float32)
    for core in range(N_CORES):
        b, s = core // 4, core % 4
        out[b, s * TOK:(s + 1) * TOK] = res.results[core]["out"]
    return out
```

```python
import math
from contextlib import ExitStack

import numpy as np
import concourse.bass as bass
import concourse.mybir as mybir
from concourse.bass_utils import run_bass_kernel_spmd

F32 = mybir.dt.float32
BF16 = mybir.dt.bfloat16
I32 = mybir.dt.int32
AF = mybir.ActivationFunctionType
ALU = mybir.AluOpType
AX = mybir.AxisListType

ENGS = ['pe', 'act', 'dve', 'pool', 'sp']
NDMA = 24

N_CORES = 8
D = 1024
KC = 8
T = 128
NT = 16
NPRE = 48
TOK = NT * T
NEXP = 256
EPS = 1e-6
NEG = -30000.0

C_GQ, C_GK, C_GV, C_GLOW, C_GR, C_SQ, C_SK, C_SV = 0, 256, 512, 1024, 1040, 1552, 2064, 2192
IN_W = 2320


class Res:
    __slots__ = ('name', 'w', 'r', 'excl')

    def __init__(self, name, excl=False):
        self.name = name
        self.w = None
        self.r = {}
        self.excl = excl


class Sched:
    def __init__(self, nc, es):
        self.nc = nc
        self.ops = {e: [] for e in ENGS}
        self.esem = {e: es.enter_context(nc.semaphore('s_' + e)) for e in ENGS}
        self.dsem = [es.enter_context(nc.semaphore('d%d' % i)) for i in range(NDMA)]
        self.dcount = [0] * NDMA
        self.dnext = 0
        self.waited = {e: {} for e in ENGS}

    def _need(self, eng, ev, waits, is_dma):
        if ev is None:
            return
        if ev[0] == 'e':
            _, E, i = ev
            if E == eng and eng == 'pe' and not is_dma:
                return
            if E == eng and eng == 'sp':
                return
            key = ('e', E)
            if self.waited[eng].get(key, -1) >= i:
                return
            self.waited[eng][key] = i
            self.ops[E][i]['signal'] = True
            waits.append(ev)
        else:
            _, k, v = ev
            key = ('d', k)
            if self.waited[eng].get(key, 0) >= v:
                return
            self.waited[eng][key] = v
            waits.append(ev)

    def op(self, eng, fn, reads=(), writes=(), dma=False):
        waits = []
        for R in reads:
            self._need(eng, R.w, waits, dma)
            if R.excl:
                for ev in R.r.values():
                    if ev[0] == 'e' and ev[1] == eng:
                        continue
                    self._need(eng, ev, waits, dma)
        for R in writes:
            self._need(eng, R.w, waits, dma)
            for ev in R.r.values():
                self._need(eng, ev, waits, dma)
        rec = {'fn': fn, 'waits': waits, 'signal': False, 'dma': None}
        idx = len(self.ops[eng])
        if dma:
            k = self.dnext
            self.dnext = (self.dnext + 1) % NDMA
            if self.dcount[k] > 0:
                self._need(eng, ('d', k, self.dcount[k]), waits, dma)
            self.dcount[k] += 16
            rec['dma'] = k
            ev = ('d', k, self.dcount[k])
        else:
            ev = ('e', eng, idx)
        self.ops[eng].append(rec)
        for R in reads:
            R.r[ev[:2]] = ev
        for R in writes:
            R.w = ev
            R.r = {}
        return ev

    def wait_all(self, eng):
        waits = []
        for E in ENGS:
            if E != eng and E != 'sp' and self.ops[E]:
                i = len(self.ops[E]) - 1
                while i >= 0 and (self.ops[E][i]['fn'] is None or self.ops[E][i]['dma'] is not None):
                    i -= 1
                if i >= 0:
                    self._need(eng, ('e', E, i), waits, False)
        for k in range(NDMA):
            if self.dcount[k] > 0:
                self._need(eng, ('d', k, self.dcount[k]), waits, False)
        self.ops[eng].append({'fn': None, 'waits': waits, 'signal': False, 'dma': None})

    def barrier(self):
        for E in ENGS:
            self.wait_all(E)

    def emit(self):
        nc = self.nc
        pref = {}
        for E in ENGS:
            c = 0
            arr = []
            for o in self.ops[E]:
                if o['signal']:
                    c += 1
                arr.append(c)
            pref[E] = arr
        stats = {}

        def run(E, eng):
            nw = 0
            for o in self.ops[E]:
                for ev in o['waits']:
                    nw += 1
                    if ev[0] == 'e':
                        eng.wait_ge(self.esem[ev[1]], pref[ev[1]][ev[2]])
                    else:
                        eng.wait_ge(self.dsem[ev[1]], ev[2])
                if o['fn'] is None:
                    continue
                if o['fn'] == 'nop':
                    inst = eng.nop()
                else:
                    inst = o['fn'](eng)
                if o['signal']:
                    inst.then_inc(self.esem[E], 1)
                if o['dma'] is not None:
                    inst.then_inc(self.dsem[o['dma']], 16)
            stats[E] = (len(self.ops[E]), nw)

        with nc.Block() as block:
            @block.tensor
            def _(eng):
                run('pe', eng)

            @block.scalar
            def _(eng):
                run('act', eng)

            @block.vector
            def _(eng):
                run('dve', eng)

            @block.gpsimd
            def _(eng):
                run('pool', eng)

            @block.sync
            def _(eng):
                run('sp', eng)
        return stats


class Buf:
    __slots__ = ('t', 'r')

    def __init__(self, t, r):
        self.t = t
        self.r = r

    def __getitem__(self, k):
        return self.t[k]


class _Stop(Exception):
    pass


def build_program(n_exp_decl=NEXP, dbg=False, n_pre=NPRE, n_main=NT, stop=None):
    nc = bass.Bass("TRN2", target_bir_lowering=False)

    def din(name, shape, dt=F32):
        return nc.dram_tensor(name, list(shape), dt, kind="ExternalInput").ap()

    x_main = din("x_main", [TOK, D])
    x_pre = din("x_pre", [NPRE * T, D])
    valid_d = din("valid", [T, NPRE])
    pos_d = din("pos", [T, NT + 1], I32)
    mask0_d = din("mask0", [T, 256])
    c_col_d = din("c_col", [T, KC])
    w_ada_d = din("w_ada", [D, 6 * D])
    b_ada_col_d = din("b_ada_col", [T, 48])
    b_ada_d = din("b_ada", [1, 6 * D])
    g_attn_col_d = din("g_attn_col", [T, KC])
    g_ffn_col_d = din("g_ffn_col", [T, KC])
    g_final_d = din("g_final", [1, D])
    w_in_d = din("w_in", [D, IN_W])
    w_gk_up_d = din("w_gk_up", [16, 256])
    b_gk_d = din("b_gk", [1, 256])
    g_gla_d = din("g_gla", [1, 128])
    sinks_d = din("sinks", [1, 8])
    w_out_d = din("w_out", [D, D])
    w_router_d = din("w_router", [D, NEXP])
    rbias_d = din("router_bias", [1, NEXP])
    w_gate_d = din("w_gate", [n_exp_decl, D, 256])
    w_up_d = din("w_up", [n_exp_decl, D, 256])
    w_down_d = din("w_down", [n_exp_decl, 256, D])
    ws_gate_d = din("ws_gate", [D, 256])
    ws_up_d = din("ws_up", [D, 256])
    ws_down_d = din("ws_down", [256, D])
    out_d = nc.dram_tensor("out", [TOK, D], F32, kind="ExternalOutput").ap()
    x1s_d = nc.dram_tensor("x1s", [TOK, D], F32, kind="Internal").ap()
    if dbg:
        d_x1 = nc.dram_tensor("d_x1", [TOK, D], F32, kind="ExternalOutput").ap()
        d_mix = nc.dram_tensor("d_mix", [TOK, D], F32, kind="ExternalOutput").ap()
        d_W = nc.dram_tensor("d_W", [TOK, NEXP + 1], F32, kind="ExternalOutput").ap()

    inv_freq = [float(np.float32(np.power(np.float32(500000.0), np.float32(-(2 * i) / 16.0)))) for i in range(8)]

    with ExitStack() as es:
        S = Sched(nc, es)

        def sbuf(stack, name, shape, dt):
            return Buf(stack.enter_context(nc.sbuf_tensor("sb_" + name, list(shape), dt)), Res(name))

        def A(eng, fn, r=(), w=(), dma=False):
            S.op(eng, fn, reads=[b.r for b in r], writes=[b.r for b in w], dma=dma)

        PB = [Buf(es.enter_context(nc.psum_tensor("pb%d" % i, [128, 512], F32)), Res("pb%d" % i, excl=True)) for i in range(8)]

        h2T = sbuf(es, "h2T", [128, KC, TOK], BF16)
        Wr = sbuf(es, "Wr", [128, NT, NEXP + 1], F32)
        gt1_b = sbuf(es, "gt1_b", [128, D], F32)
        gt2_b = sbuf(es, "gt2_b", [128, D], F32)
        gfin_b = sbuf(es, "gfin_b", [128, D], F32)
        identf = sbuf(es, "identf", [128, 128], F32)
        ident = sbuf(es, "ident", [128, 128], BF16)
        triT = sbuf(es, "triT", [128, 128], F32)
        ustr = sbuf(es, "ustr", [128, 128], F32)
        ones_f = sbuf(es, "ones_f", [128, 128], F32)
        maskb = sbuf(es, "maskb", [128, 256], F32)
        mask0 = sbuf(es, "mask0", [128, 256], F32)
        modcol = sbuf(es, "modcol", [128, 32], F32)
        s1c = sbuf(es, "s1c", [128, KC], F32)
        s2c = sbuf(es, "s2c", [128, KC], F32)
        rbias_b = sbuf(es, "rbias_b", [128, NEXP], F32)
        ggla_b = sbuf(es, "ggla_b", [128, 512], F32)
        sink_b = sbuf(es, "sink_b", [128, 8], F32)
        cosA = sbuf(es, "cosA", [128, NT + 1, 8], F32)
        sinA = sbuf(es, "sinA", [128, NT + 1, 8], F32)
        valid = sbuf(es, "valid", [128, NPRE], F32)

        try:
            A('pool', lambda e: e.memset(ones_f[:], 1.0), w=[ones_f])
            A('pool', lambda e: e.memset(identf[:], 1.0), w=[identf])
            A('pool', lambda e: e.affine_select(out=identf[:], in_=identf[:], pattern=[[1, 128]], compare_op=ALU.is_equal, fill=0.0, base=0, channel_multiplier=-1), r=[identf], w=[identf])
            A('dve', lambda e: e.tensor_copy(out=ident[:], in_=identf[:]), r=[identf], w=[ident])
            A('pool', lambda e: e.affine_select(out=triT[:], in_=ones_f[:], pattern=[[1, 128]], compare_op=ALU.is_ge, fill=0.0, base=0, channel_multiplier=-1), r=[ones_f], w=[triT])
            A('pool', lambda e: e.affine_select(out=ustr[:], in_=ones_f[:], pattern=[[-1, 128]], compare_op=ALU.is_ge, fill=0.0, base=-1, channel_multiplier=1), r=[ones_f], w=[ustr])
            A('pool', lambda e: e.memset(maskb[:], 0.0), w=[maskb])
            A('pool', lambda e: e.affine_select(out=maskb[:, 0:128], in_=maskb[:, 0:128], pattern=[[1, 128]], compare_op=ALU.is_ge, fill=NEG, base=-1, channel_multiplier=-1), r=[maskb], w=[maskb])
            A('pool', lambda e: e.affine_select(out=maskb[:, 128:256], in_=maskb[:, 128:256], pattern=[[-1, 128]], compare_op=ALU.is_ge, fill=NEG, base=0, channel_multiplier=1), r=[maskb], w=[maskb])
            A('sp', lambda e: e.dma_start(out=mask0[:], in_=mask0_d), w=[mask0], dma=True)
            A('sp', lambda e: e.dma_start(out=valid[:], in_=valid_d), w=[valid], dma=True)
            A('sp', lambda e: e.dma_start(out=rbias_b[:], in_=rbias_d.partition_broadcast(128)), w=[rbias_b], dma=True)
            A('sp', lambda e: e.dma_start(out=sink_b[:], in_=sinks_d.partition_broadcast(128)), w=[sink_b], dma=True)
            A('sp', lambda e: e.dma_start(out=gfin_b[:], in_=g_final_d.partition_broadcast(128)), w=[gfin_b], dma=True)
            for h in range(4):
                A('sp', lambda e, h=h: e.dma_start(out=ggla_b[:, h * 128:(h + 1) * 128], in_=g_gla_d.partition_broadcast(128)), w=[ggla_b], dma=True)
            A('sp', lambda e: e.dma_start(out=gt1_b[:], in_=b_ada_d[:, 2 * D:3 * D].partition_broadcast(128)), w=[gt1_b], dma=True)
            A('sp', lambda e: e.dma_start(out=gt2_b[:], in_=b_ada_d[:, 5 * D:6 * D].partition_broadcast(128)), w=[gt2_b], dma=True)
            A('pool', lambda e: e.memset(Wr[:, :, NEXP:NEXP + 1], 1.0), w=[Wr])
            hmk = sbuf(es, 'hmk', [128, 4], F32)
            A('pool', lambda e: e.memset(hmk[:], 0.0), w=[hmk])
            A('pool', lambda e: e.memset(hmk[0:64, 0:1], 1.0), r=[hmk], w=[hmk])
            A('pool', lambda e: e.memset(hmk[64:128, 1:2], 1.0), r=[hmk], w=[hmk])
            A('pool', lambda e: e.memset(hmk[0:64, 2:3], 0.125), r=[hmk], w=[hmk])
            A('pool', lambda e: e.memset(hmk[64:128, 3:4], 0.125), r=[hmk], w=[hmk])

            if stop == 'const':
                raise _Stop()
            with ExitStack() as p0:
                posi = sbuf(p0, "posi", [128, NT + 1], I32)
                posf = sbuf(p0, "posf", [128, NT + 1], F32)
                ang = sbuf(p0, "ang", [128, 2, NT + 1, 8], F32)
                kf = sbuf(p0, "kf", [128, 2, NT + 1, 8], F32)
                ki = sbuf(p0, "ki", [128, 2, NT + 1, 8], I32)
                A('sp', lambda e: e.dma_start(out=posi[:], in_=pos_d), w=[posi], dma=True)
                A('dve', lambda e: e.tensor_copy(out=posf[:], in_=posi[:]), r=[posi], w=[posf])
                for f in range(8):
                    A('dve', lambda e, f=f: e.tensor_scalar(out=ang[:, 0, :, f], in0=posf[:], scalar1=inv_freq[f], scalar2=None, op0=ALU.mult), r=[posf], w=[ang])
                A('dve', lambda e: e.tensor_scalar(out=ang[:, 1], in0=ang[:, 0], scalar1=math.pi / 2, scalar2=None, op0=ALU.add), r=[ang], w=[ang])
                A('dve', lambda e: e.tensor_scalar(out=kf[:], in0=ang[:], scalar1=1.0 / (2 * math.pi), scalar2=None, op0=ALU.mult), r=[ang], w=[kf])
                A('dve', lambda e: e.tensor_copy(out=ki[:], in_=kf[:]), r=[kf], w=[ki])
                A('dve', lambda e: e.tensor_copy(out=kf[:], in_=ki[:]), r=[ki], w=[kf])
                A('dve', lambda e: e.scalar_tensor_tensor(out=ang[:], in0=kf[:], scalar=-2 * math.pi, in1=ang[:], op0=ALU.mult, op1=ALU.add), r=[kf, ang], w=[ang])
                A('dve', lambda e: e.tensor_single_scalar(out=kf[:], in_=ang[:], scalar=math.pi, op=ALU.is_gt), r=[ang], w=[kf])
                A('dve', lambda e: e.scalar_tensor_tensor(out=ang[:], in0=kf[:], scalar=-2 * math.pi, in1=ang[:], op0=ALU.mult, op1=ALU.add), r=[kf, ang], w=[ang])
                A('dve', lambda e: e.tensor_single_scalar(out=kf[:], in_=ang[:], scalar=-math.pi, op=ALU.is_lt), r=[ang], w=[kf])
                A('dve', lambda e: e.scalar_tensor_tensor(out=ang[:], in0=kf[:], scalar=2 * math.pi, in1=ang[:], op0=ALU.mult, op1=ALU.add), r=[kf, ang], w=[ang])
                A('act', lambda e: e.activation(out=sinA[:], in_=ang[:, 0], func=AF.Sin), r=[ang], w=[sinA])
                A('act', lambda e: e.activation(out=cosA[:], in_=ang[:, 1], func=AF.Sin), r=[ang], w=[cosA])

                if stop == 'rope':
                    raise _Stop()
                c_col = sbuf(p0, "c_col", [128, KC], F32)
                cact = sbuf(p0, "cact", [128, KC], F32)
                crep = sbuf(p0, "crep", [128, KC, 128], F32)
                bcol = sbuf(p0, "bcol", [128, 48], F32)
                gac = sbuf(p0, "gac", [128, KC], F32)
                gfc = sbuf(p0, "gfc", [128, KC], F32)
                wa = [sbuf(p0, "wa%d" % i, [128, KC, 512], F32) for i in range(2)]
                A('sp', lambda e: e.dma_start(out=c_col[:], in_=c_col_d), w=[c_col], dma=True)
                A('sp', lambda e: e.dma_start(out=bcol[:], in_=b_ada_col_d), w=[bcol], dma=True)
                A('sp', lambda e: e.dma_start(out=gac[:], in_=g_attn_col_d), w=[gac], dma=True)
                A('sp', lambda e: e.dma_start(out=gfc[:], in_=g_ffn_col_d), w=[gfc], dma=True)
                A('act', lambda e: e.activation(out=cact[:], in_=c_col[:], func=AF.Silu), r=[c_col], w=[cact])
                for k in range(KC):
                    A('dve', lambda e, k=k: e.tensor_scalar(out=crep[:, k, :], in0=ones_f[:], scalar1=cact[:, k:k + 1], scalar2=None, op0=ALU.mult), r=[ones_f, cact], w=[crep])
                colq = 0
                for j in range(12):
                    wb = wa[j % 2]
                    A('sp', lambda e, j=j, wb=wb: e.dma_start(out=wb[:], in_=w_ada_d[:, j * 512:(j + 1) * 512].rearrange("(c p) f -> p c f", p=128)), w=[wb], dma=True)
                    if j in (4, 5, 10, 11):
                        dst = gt1_b if j in (4, 5) else gt2_b
                        half = j % 2
                        pbk = PB[1 + half]
                        for k in range(KC):
                            A('pe', lambda e, k=k, wb=wb, pbk=pbk: e.matmul(pbk[:], lhsT=crep[:, k, :], rhs=wb[:, k, :], start=(k == 0), stop=(k == KC - 1)), r=[crep, wb], w=[pbk])
                        A('dve', lambda e, dst=dst, half=half, pbk=pbk: e.tensor_tensor(out=dst[:, half * 512:(half + 1) * 512], in0=pbk[:], in1=dst[:, half * 512:(half + 1) * 512], op=ALU.add), r=[pbk, dst], w=[dst])
                    else:
                        for q in range(4):
                            for k in range(KC):
                                A('pe', lambda e, k=k, q=q, wb=wb, cq=colq: e.matmul(PB[0][:, cq:cq + 1], lhsT=wb[:, k, q * 128:(q + 1) * 128], rhs=cact[:, k:k + 1], start=(k == 0), stop=(k == KC - 1)), r=[cact, wb], w=[PB[0]])
                            colq += 1
                A('dve', lambda e: e.tensor_tensor(out=modcol[:, 0:16], in0=PB[0][:, 0:16], in1=bcol[:, 0:16], op=ALU.add), r=[PB[0], bcol], w=[modcol])
                A('dve', lambda e: e.tensor_tensor(out=modcol[:, 16:32], in0=PB[0][:, 16:32], in1=bcol[:, 24:40], op=ALU.add), r=[PB[0], bcol], w=[modcol])
                A('dve', lambda e: e.scalar_tensor_tensor(out=s1c[:], in0=modcol[:, 8:16], scalar=1.0, in1=gac[:], op0=ALU.add, op1=ALU.mult), r=[modcol, gac], w=[s1c])
                A('dve', lambda e: e.scalar_tensor_tensor(out=s2c[:], in0=modcol[:, 24:32], scalar=1.0, in1=gfc[:], op0=ALU.add, op1=ALU.mult), r=[modcol, gfc], w=[s2c])
                S.barrier()
            if stop == 'ada':
                raise _Stop()

            with ExitStack() as p1:
                w_in = sbuf(p1, "w_in", [128, KC, IN_W], BF16)
                w_out = sbuf(p1, "w_out", [128, KC, D], BF16)
                w_rt = sbuf(p1, "w_rt", [128, KC, NEXP], BF16)
                wgk = sbuf(p1, "wgk", [16, 256], F32)
                bgk = sbuf(p1, "bgk", [1, 256], F32)
                stg = sbuf(p1, "stg", [128, IN_W], F32)
                for k in range(KC):
                    A('sp', lambda e, k=k: e.dma_start(out=stg[:], in_=w_in_d[k * 128:(k + 1) * 128, :]), w=[stg], dma=True)
                    A('pool', lambda e, k=k: e.tensor_copy(out=w_in[:, k, :], in_=stg[:]), r=[stg], w=[w_in])
                for k in range(KC):
                    A('sp', lambda e, k=k: e.dma_start(out=stg[:, 0:D], in_=w_out_d[k * 128:(k + 1) * 128, :]), w=[stg], dma=True)
                    A('pool', lambda e, k=k: e.tensor_copy(out=w_out[:, k, :], in_=stg[:, 0:D]), r=[stg], w=[w_out])
                A('sp', lambda e: e.dma_start(out=stg[:, 0:KC * NEXP].rearrange("p (c f) -> p c f", c=KC), in_=w_router_d.rearrange("(c p) f -> p c f", p=128)), w=[stg], dma=True)
                A('pool', lambda e: e.tensor_copy(out=w_rt[:].rearrange("p c f -> p (c f)"), in_=stg[:, 0:KC * NEXP]), r=[stg], w=[w_rt])
                A('sp', lambda e: e.dma_start(out=wgk[:], in_=w_gk_up_d), w=[wgk], dma=True)
                A('sp', lambda e: e.dma_start(out=bgk[:], in_=b_gk_d), w=[bgk], dma=True)

                xt = sbuf(p1, "xt", [128, D], F32)
                sqj = sbuf(p1, "sqj", [128, D], F32)
                ss = sbuf(p1, "ss", [128, 4], F32)
                xs = sbuf(p1, "xs", [128, D], BF16)
                hT = sbuf(p1, "hT", [128, KC, 128], BF16)
                glowT = sbuf(p1, "glowT", [16, 128], F32)
                ab = sbuf(p1, "ab", [128, 256], F32)
                ex = sbuf(p1, "ex", [128, 256], F32)
                la = sbuf(p1, "la", [128, 256], F32)
                E1 = sbuf(p1, "E1", [128, 256], F32)
                E2 = sbuf(p1, "E2", [128, 256], F32)
                E3 = sbuf(p1, "E3", [128, 256], F32)
                decc = sbuf(p1, "decc", [128, 2], F32)
                qdT = sbuf(p1, "qdT", [128, 4, 128], BF16)
                kdT = sbuf(p1, "kdT", [128, 4, 128], BF16)
                kdec = sbuf(p1, "kdec", [128, 256], BF16)
                v_bf = sbuf(p1, "v_bf", [128, 512], BF16)
                attnT = sbuf(p1, "attnT", [128, 4, 128], BF16)
                Sst = sbuf(p1, "Sst", [128, 2, 128], F32)
                S_bf = sbuf(p1, "S_bf", [128, 2, 128], BF16)
                ssg = sbuf(p1, "ssg", [128, 4], F32)
                rstdg = sbuf(p1, "rstdg", [128, 4], F32)
                gsil = sbuf(p1, "gsil", [128, 512], F32)
                mix = sbuf(p1, "mix", [128, D], BF16)
                mixT = sbuf(p1, "mixT", [128, KC, 128], BF16)
                q_r = sbuf(p1, "q_r", [128, 4, 2, 64], BF16)
                rt1 = sbuf(p1, "rt1", [128, 4, 2, 8], F32)
                rt2 = sbuf(p1, "rt2", [128, 4, 2, 8], F32)
                kv_r = [sbuf(p1, "kv_r%d" % i, [128, 256], BF16) for i in range(2)]
                kT = [sbuf(p1, "kT%d" % i, [128, 128], BF16) for i in range(2)]
                qT = sbuf(p1, "qT", [128, 2, 4, 128], BF16)
                sm = sbuf(p1, "sm", [128, 4, 256], F32)
                rmax = sbuf(p1, "rmax", [128, 8], F32)
                negm = sbuf(p1, "negm", [128, 8], F32)
                rsum = sbuf(p1, "rsum", [128, 8], F32)
                esk = sbuf(p1, "esk", [128, 8], F32)
                pb = sbuf(p1, "pb", [128, 4, 256], BF16)
                pT = sbuf(p1, "pT", [128, 8, 128], BF16)
                x1 = sbuf(p1, "x1", [128, D], F32)
                tmpm = sbuf(p1, "tmpm", [128, D], F32)
                sc = sbuf(p1, "sc", [128, NEXP], F32)
                sel = sbuf(p1, "sel", [128, NEXP], F32)
                sel2 = sbuf(p1, "sel2", [128, NEXP], F32)
                eqm = sbuf(p1, "eqm", [128, NEXP], F32)
                m1 = sbuf(p1, "m1", [128, 8], F32)
                m2 = sbuf(p1, "m2", [128, 8], F32)
                gs = sbuf(p1, "gs", [128, 8], F32)
                top8 = sbuf(p1, "top8", [128, 8], F32)
                pen = sbuf(p1, "pen", [128, 8], F32)
                wsum = sbuf(p1, "wsum", [128, 1], F32)

                A('dve', lambda e: e.memset(Sst[:], 0.0), w=[Sst])
                A('dve', lambda e: e.memset(S_bf[:], 0.0), w=[S_bf])

                def bfv(pbuf):
                    return pbuf[:].bitcast(BF16)

                def front(xsrc, scol, bcol_, hT_dst):
                    A('sp', lambda e: e.dma_start(out=xt[:], in_=xsrc), w=[xt], dma=True)
                    front_from_sbuf(xt, scol, bcol_, hT_dst)

                def front_from_sbuf(xsb, scol, bcol_, hT_dst, hT_buf=None):
                    hb = hT if hT_buf is None else hT_buf
                    A('act', lambda e: e.activation(out=sqj[:], in_=xsb[:], func=AF.Square, accum_out=ss[:, 0:1]), r=[xsb], w=[sqj, ss])
                    A('act', lambda e: e.activation(out=ss[:, 1:2], in_=ss[:, 0:1], func=AF.Sqrt, scale=1.0 / D, bias=EPS), r=[ss], w=[ss])
                    A('dve', lambda e: e.reciprocal(out=ss[:, 2:3], in_=ss[:, 1:2]), r=[ss], w=[ss])
                    A('dve', lambda e: e.tensor_scalar(out=xs[:], in0=xsb[:], scalar1=ss[:, 2:3], scalar2=None, op0=ALU.mult), r=[xsb, ss], w=[xs])
                    pv = bfv(PB[0])
                    for c in range(KC):
                        A('pe', lambda e, c=c: e.transpose(out=pv[:, c * 128:(c + 1) * 128], in_=xs[:, c * 128:(c + 1) * 128], identity=ident[:]), r=[xs, ident], w=[PB[0]])
                    for c in range(KC):
                        A('act', lambda e, c=c: e.activation(out=hT_dst(c), in_=pv[:, c * 128:(c + 1) * 128], func=AF.Identity, scale=scol[:, c:c + 1], bias=bcol_[:, c:c + 1]), r=[PB[0], scol, bcol_], w=[hb])

                def proj_tok(pbk, col0, col1, c_lo, c_hi):
                    n = c_hi - c_lo
                    for k in range(KC):
                        A('pe', lambda e, k=k: e.matmul(pbk[:, col0:col0 + n], lhsT=hT[:, k, :], rhs=w_in[:, k, c_lo:c_hi], start=(k == 0), stop=(k == KC - 1)), r=[hT, w_in], w=[pbk])

                def proj_feat(pbk, col0, c_lo, m):
                    for k in range(KC):
                        A('pe', lambda e, k=k: e.matmul(pbk[0:m, col0:col0 + 128], lhsT=w_in[:, k, c_lo:c_lo + m], rhs=hT[:, k, :], start=(k == 0), stop=(k == KC - 1)), r=[hT, w_in], w=[pbk])

                def gate_logs():
                    A('dve', lambda e: e.tensor_copy(out=glowT[:], in_=PB[6][0:16, 0:128]), r=[PB[6]], w=[glowT])
                    A('pe', lambda e: e.matmul(PB[6][:, 128:384], lhsT=glowT[:], rhs=wgk[:], start=True, stop=False), r=[glowT, wgk], w=[PB[6]])
                    A('pe', lambda e: e.matmul(PB[6][:, 128:384], lhsT=ones_f[0:1, :], rhs=bgk[:], start=False, stop=True), r=[ones_f, bgk], w=[PB[6]])
                    pre = PB[6][:, 128:384]
                    A('act', lambda e: e.activation(out=ab[:], in_=pre, func=AF.Abs), r=[PB[6]], w=[ab])
                    A('act', lambda e: e.activation(out=ex[:], in_=ab[:], func=AF.Exp, scale=-1.0), r=[ab], w=[ex])
                    A('act', lambda e: e.activation(out=ex[:], in_=ex[:], func=AF.Ln, bias=1.0), r=[ex], w=[ex])
                    A('dve', lambda e: e.scalar_tensor_tensor(out=la[:], in0=pre, scalar=0.0, in1=ex[:], op0=ALU.min, op1=ALU.subtract), r=[PB[6], ex], w=[la])
                    A('dve', lambda e: e.tensor_scalar(out=la[:], in0=la[:], scalar1=1.0 / 16.0, scalar2=None, op0=ALU.mult), r=[la], w=[la])

                def state_update():
                    for p in range(2):
                        A('pe', lambda e, p=p: e.matmul(PB[7][:, p * 256:(p + 1) * 256], lhsT=kdec[:, p * 128:(p + 1) * 128], rhs=v_bf[:, p * 256:(p + 1) * 256], start=True, stop=True), r=[kdec, v_bf], w=[PB[7]])
                    for p in range(2):
                        for w_ in range(2):
                            rs = slice(w_ * 64, (w_ + 1) * 64)
                            A('dve', lambda e, p=p, w_=w_, rs=rs: e.scalar_tensor_tensor(out=Sst[rs, p, :], in0=Sst[rs, p, :], scalar=decc[rs, p:p + 1], in1=PB[7][rs, p * 256 + w_ * 128:p * 256 + (w_ + 1) * 128], op0=ALU.mult, op1=ALU.add), r=[Sst, decc, PB[7]], w=[Sst])
                    A('act', lambda e: e.copy(out=S_bf[:], in_=Sst[:]), r=[Sst], w=[S_bf])

                def prefix_tile(t, last):
                    front(x_pre[t * 128:(t + 1) * 128, :], s1c, modcol_sh1, lambda c: hT[:, c, :])
                    proj_tok(PB[1], 0, 512, C_GV, C_GV + 512)
                    if last:
                        proj_tok(PB[4], 0, 256, C_SK, C_SK + 256)
                        proj_tok(PB[4], 256, 512, C_GK, C_GK + 256)
                    else:
                        proj_tok(PB[4], 256, 512, C_GK, C_GK + 256)
                    proj_feat(PB[6], 0, C_GLOW, 16)
                    gate_logs()
                    A('pe', lambda e: e.matmul(PB[7][:, 256:512], lhsT=ustr[:], rhs=la[:], start=True, stop=True), r=[ustr, la], w=[PB[7]])
                    for p in range(2):
                        A('pe', lambda e, p=p: e.matmul(PB[7][:, p:p + 1], lhsT=la[:, p * 128:(p + 1) * 128], rhs=ones_f[:, 0:1], start=True, stop=True), r=[la, ones_f], w=[PB[7]])
                    A('act', lambda e: e.activation(out=E3[:], in_=PB[7][:, 256:512], func=AF.Exp), r=[PB[7]], w=[E3])
                    A('act', lambda e: e.activation(out=decc[:], in_=PB[7][:, 0:2], func=AF.Exp), r=[PB[7]], w=[decc])
                    A('dve', lambda e: e.scalar_tensor_tensor(out=kdec[:], in0=PB[4][:, 256:512], scalar=valid[:, t:t + 1], in1=E3[:], op0=ALU.mult, op1=ALU.mult), r=[PB[4], valid, E3], w=[kdec])
                    A('act', lambda e: e.copy(out=v_bf[:], in_=PB[1][:]), r=[PB[1]], w=[v_bf])
                    if last:
                        rope_kv(0, 0)
                    state_update()

                def rope_kv(slot, tcol):
                    kvb = kv_r[slot]
                    A('dve', lambda e: e.tensor_copy(out=kvb[:], in_=PB[4][:, 0:256]), r=[PB[4]], w=[kvb])
                    kv3 = PB[4][:, 0:128].rearrange("p (h d) -> p h d", h=2)
                    ko3 = kvb[:, 0:128].rearrange("p (h d) -> p h d", h=2)
                    cb = cosA[:, tcol, :].unsqueeze(1).to_broadcast([128, 2, 8])
                    sb_ = sinA[:, tcol, :].unsqueeze(1).to_broadcast([128, 2, 8])
                    r1 = rt1[:, 0, :, :]
                    r2 = rt2[:, 0, :, :]
                    A('dve', lambda e: e.tensor_tensor(out=r1, in0=kv3[:, :, 0:8], in1=cb, op=ALU.mult), r=[PB[4], cosA], w=[rt1])
                    A('dve', lambda e: e.tensor_tensor(out=r2, in0=kv3[:, :, 8:16], in1=sb_, op=ALU.mult), r=[PB[4], sinA], w=[rt2])
                    A('dve', lambda e: e.tensor_tensor(out=ko3[:, :, 0:8], in0=r1, in1=r2, op=ALU.subtract), r=[rt1, rt2], w=[kvb])
                    A('dve', lambda e: e.tensor_tensor(out=r1, in0=kv3[:, :, 8:16], in1=cb, op=ALU.mult), r=[PB[4], cosA], w=[rt1])
                    A('dve', lambda e: e.tensor_tensor(out=r2, in0=kv3[:, :, 0:8], in1=sb_, op=ALU.mult), r=[PB[4], sinA], w=[rt2])
                    A('dve', lambda e: e.tensor_tensor(out=ko3[:, :, 8:16], in0=r1, in1=r2, op=ALU.add), r=[rt1, rt2], w=[kvb])
                    pv = bfv(PB[0])
                    A('pe', lambda e: e.transpose(out=pv[:, 0:128], in_=kvb[:, 0:128], identity=ident[:]), r=[kvb, ident], w=[PB[0]])
                    A('act', lambda e: e.copy(out=kT[slot][:], in_=pv[:, 0:128]), r=[PB[0]], w=[kT[slot]])

                modcol_sh1 = Buf(modcol.t[:, 0:8], modcol.r)
                modcol_sh2 = Buf(modcol.t[:, 16:24], modcol.r)

                for t in range(NPRE - n_pre, NPRE):
                    prefix_tile(t, t == NPRE - 1)

                def main_tile(i):
                    cur = (i + 1) % 2
                    prv = i % 2
                    tcol = i + 1
                    front(x_main[i * 128:(i + 1) * 128, :], s1c, modcol_sh1, lambda c: hT[:, c, :])
                    proj_tok(PB[1], 0, 512, C_GV, C_GV + 512)
                    proj_tok(PB[2], 0, 512, C_GR, C_GR + 512)
                    proj_tok(PB[3], 0, 512, C_SQ, C_SQ + 512)
                    proj_tok(PB[4], 0, 256, C_SK, C_SK + 256)
                    proj_tok(PB[4], 256, 512, C_GK, C_GK + 256)
                    for p in range(2):
                        proj_feat(PB[5], p * 128, C_GQ + p * 128, 128)
                    for p in range(2):
                        proj_feat(PB[5], 256 + p * 128, C_GK + p * 128, 128)
                    proj_feat(PB[6], 0, C_GLOW, 16)
                    gate_logs()
                    for p in range(2):
                        A('pe', lambda e, p=p: e.matmul(PB[7][:, p * 128:(p + 1) * 128], lhsT=la[:, p * 128:(p + 1) * 128], rhs=triT[:], start=True, stop=True), r=[la, triT], w=[PB[7]])
                    A('pe', lambda e: e.matmul(PB[7][:, 256:512], lhsT=ustr[:], rhs=la[:], start=True, stop=True), r=[ustr, la], w=[PB[7]])
                    A('act', lambda e: e.activation(out=E1[:], in_=PB[7][:, 0:256], func=AF.Exp), r=[PB[7]], w=[E1])
                    A('act', lambda e: e.activation(out=E2[:], in_=PB[7][:, 0:256], func=AF.Exp, scale=-1.0), r=[PB[7]], w=[E2])
                    A('act', lambda e: e.activation(out=E3[:], in_=PB[7][:, 256:512], func=AF.Exp), r=[PB[7]], w=[E3])
                    A('dve', lambda e: e.tensor_copy(out=decc[:, 0:1], in_=E1[:, 127:128]), r=[E1], w=[decc])
                    A('dve', lambda e: e.tensor_copy(out=decc[:, 1:2], in_=E1[:, 255:256]), r=[E1], w=[decc])
                    for h_ in range(4):
                        p_, w__ = h_ // 2, h_ % 2
                        A('dve', lambda e, h_=h_, p_=p_, w__=w__: e.scalar_tensor_tensor(out=qdT[:, h_, :], in0=PB[5][:, p_ * 128:(p_ + 1) * 128], scalar=hmk[:, 2 + w__:3 + w__], in1=E1[:, p_ * 128:(p_ + 1) * 128], op0=ALU.mult, op1=ALU.mult), r=[PB[5], E1, hmk], w=[qdT])
                    for h_ in range(4):
                        p_, w__ = h_ // 2, h_ % 2
                        A('dve', lambda e, h_=h_, p_=p_, w__=w__: e.scalar_tensor_tensor(out=kdT[:, h_, :], in0=PB[5][:, 256 + p_ * 128:256 + (p_ + 1) * 128], scalar=hmk[:, w__:w__ + 1], in1=E2[:, p_ * 128:(p_ + 1) * 128], op0=ALU.mult, op1=ALU.mult), r=[PB[5], E2, hmk], w=[kdT])
                    A('dve', lambda e: e.scalar_tensor_tensor(out=kdec[:], in0=PB[4][:, 256:512], scalar=1.0, in1=E3[:], op0=ALU.mult, op1=ALU.mult), r=[PB[4], E3], w=[kdec])
                    A('act', lambda e: e.copy(out=v_bf[:], in_=PB[1][:]), r=[PB[1]], w=[v_bf])
                    for h in range(4):
                        p, w_ = h // 2, h % 2
                        rs = slice(w_ * 64, (w_ + 1) * 64)
                        A('pe', lambda e, h=h, p=p, rs=rs: e.matmul(PB[5][:, h * 128:(h + 1) * 128], lhsT=kdT[:, h, :], rhs=qdT[:, h, :], start=True, stop=True), r=[kdT, qdT], w=[PB[5]])
                    for h in range(4):
                        A('dve', lambda e, h=h: e.scalar_tensor_tensor(out=attnT[:, h, :], in0=PB[5][:, h * 128:(h + 1) * 128], scalar=1.0, in1=triT[:], op0=ALU.mult, op1=ALU.mult), r=[PB[5], triT], w=[attnT])
                    for h in range(4):
                        p, w_ = h // 2, h % 2
                        rs = slice(w_ * 64, (w_ + 1) * 64)
                        A('pe', lambda e, h=h: e.matmul(PB[1][:, h * 128:(h + 1) * 128], lhsT=attnT[:, h, :], rhs=v_bf[:, h * 128:(h + 1) * 128], start=True, stop=False), r=[attnT, v_bf], w=[PB[1]])
                        A('pe', lambda e, h=h, p=p, rs=rs: e.matmul(PB[1][:, h * 128:(h + 1) * 128], lhsT=qdT[:, h, :], rhs=S_bf[:, p, :], start=False, stop=True), r=[qdT, S_bf], w=[PB[1]])
                    state_update()
                    for h in range(4):
                        A('act', lambda e, h=h: e.activation(out=sqj[:, h * 128:(h + 1) * 128], in_=PB[1][:, h * 128:(h + 1) * 128], func=AF.Square, accum_out=ssg[:, h:h + 1]), r=[PB[1]], w=[sqj, ssg])
                    A('act', lambda e: e.activation(out=ssg[:], in_=ssg[:], func=AF.Sqrt, scale=1.0 / 128, bias=EPS), r=[ssg], w=[ssg])
                    A('dve', lambda e: e.reciprocal(out=rstdg[:], in_=ssg[:]), r=[ssg], w=[rstdg])
                    A('act', lambda e: e.activation(out=gsil[:], in_=PB[2][:], func=AF.Silu), r=[PB[2]], w=[gsil])
                    A('dve', lambda e: e.tensor_tensor(out=gsil[:], in0=gsil[:], in1=ggla_b[:], op=ALU.mult), r=[gsil, ggla_b], w=[gsil])
                    for h in range(4):
                        A('dve', lambda e, h=h: e.scalar_tensor_tensor(out=mix[:, h * 128:(h + 1) * 128], in0=PB[1][:, h * 128:(h + 1) * 128], scalar=rstdg[:, h:h + 1], in1=gsil[:, h * 128:(h + 1) * 128], op0=ALU.mult, op1=ALU.mult), r=[PB[1], rstdg, gsil], w=[mix])
                    rope_kv(cur, tcol)
                    q4 = PB[3][:].rearrange("p (w a d) -> p a w d", w=2, a=4)
                    A('act', lambda e: e.copy(out=q_r[:], in_=q4), r=[PB[3]], w=[q_r])
                    cb = cosA[:, tcol, :].unsqueeze(1).unsqueeze(1).to_broadcast([128, 4, 2, 8])
                    sb_ = sinA[:, tcol, :].unsqueeze(1).unsqueeze(1).to_broadcast([128, 4, 2, 8])
                    A('dve', lambda e: e.tensor_tensor(out=rt1[:], in0=q4[:, :, :, 0:8], in1=cb, op=ALU.mult), r=[PB[3], cosA], w=[rt1])
                    A('dve', lambda e: e.tensor_tensor(out=rt2[:], in0=q4[:, :, :, 8:16], in1=sb_, op=ALU.mult), r=[PB[3], sinA], w=[rt2])
                    A('dve', lambda e: e.tensor_tensor(out=q_r[:, :, :, 0:8], in0=rt1[:], in1=rt2[:], op=ALU.subtract), r=[rt1, rt2], w=[q_r])
                    A('dve', lambda e: e.tensor_tensor(out=rt1[:], in0=q4[:, :, :, 8:16], in1=cb, op=ALU.mult), r=[PB[3], cosA], w=[rt1])
                    A('dve', lambda e: e.tensor_tensor(out=rt2[:], in0=q4[:, :, :, 0:8], in1=sb_, op=ALU.mult), r=[PB[3], sinA], w=[rt2])
                    A('dve', lambda e: e.tensor_tensor(out=q_r[:, :, :, 8:16], in0=rt1[:], in1=rt2[:], op=ALU.add), r=[rt1, rt2], w=[q_r])
                    pv0 = bfv(PB[0])
                    for a in range(4):
                        A('pe', lambda e, a=a: e.transpose(out=pv0[:, a * 128:(a + 1) * 128], in_=q_r[:, a, :, :].rearrange("p w d -> p (w d)"), identity=ident[:]), r=[q_r, ident], w=[PB[0]])
                    for w__ in range(2):
                        A('dve', lambda e, w__=w__: e.tensor_scalar(out=qT[:, w__, :, :].rearrange("p a t -> p (a t)"), in0=pv0[:, 0:512], scalar1=hmk[:, w__:w__ + 1], scalar2=None, op0=ALU.mult), r=[PB[0], hmk], w=[qT])
                    mk = mask0 if i == 0 else maskb
                    for g in range(2):
                        banks = (PB[3], PB[4]) if g == 0 else (PB[2], PB[6])
                        for ai in range(2):
                            a = g * 2 + ai
                            bk = banks[ai]
                            for w_ in range(2):
                                rs = slice(w_ * 64, (w_ + 1) * 64)
                                A('pe', lambda e, a=a, w_=w_, rs=rs, bk=bk: e.matmul(bk[:, w_ * 256:w_ * 256 + 128], lhsT=qT[:, w_, a, :], rhs=kT[prv][:, :], start=True, stop=True), r=[qT, kT[prv]], w=[bk])
                                A('pe', lambda e, a=a, w_=w_, rs=rs, bk=bk: e.matmul(bk[:, w_ * 256 + 128:w_ * 256 + 256], lhsT=qT[:, w_, a, :], rhs=kT[cur][:, :], start=True, stop=True), r=[qT, kT[cur]], w=[bk])
                        for ai in range(2):
                            bk = banks[ai]
                            for w_ in range(2):
                                j = ai * 2 + w_
                                A('dve', lambda e, j=j, w_=w_, bk=bk, mk=mk: e.scalar_tensor_tensor(out=sm[:, j, :], in0=bk[:, w_ * 256:(w_ + 1) * 256], scalar=0.125, in1=mk[:], op0=ALU.mult, op1=ALU.add), r=[bk, mk], w=[sm])
                        gc = slice(g * 4, g * 4 + 4)
                        A('dve', lambda e, gc=gc: e.tensor_reduce(out=rmax[:, gc], in_=sm[:], axis=AX.X, op=ALU.max), r=[sm], w=[rmax])
                        for ai in range(2):
                            for w_ in range(2):
                                j = g * 4 + ai * 2 + w_
                                hq = w_ * 4 + g * 2 + ai
                                A('dve', lambda e, j=j, hq=hq: e.tensor_tensor(out=rmax[:, j:j + 1], in0=rmax[:, j:j + 1], in1=sink_b[:, hq:hq + 1], op=ALU.max), r=[rmax, sink_b], w=[rmax])
                                A('dve', lambda e, j=j, hq=hq: e.tensor_tensor(out=esk[:, j:j + 1], in0=sink_b[:, hq:hq + 1], in1=rmax[:, j:j + 1], op=ALU.subtract), r=[rmax, sink_b], w=[esk])
                        A('dve', lambda e, gc=gc: e.tensor_scalar(out=negm[:, gc], in0=rmax[:, gc], scalar1=-1.0, scalar2=None, op0=ALU.mult), r=[rmax], w=[negm])
                        for j in range(4):
                            A('act', lambda e, j=j, g=g: e.activation(out=pb[:, j, :], in_=sm[:, j, :], func=AF.Exp, bias=negm[:, g * 4 + j:g * 4 + j + 1], accum_out=rsum[:, g * 4 + j:g * 4 + j + 1]), r=[sm, negm], w=[pb, rsum])
                        A('act', lambda e, gc=gc: e.activation(out=esk[:, gc], in_=esk[:, gc], func=AF.Exp), r=[esk], w=[esk])
                        A('dve', lambda e, gc=gc: e.tensor_tensor(out=rsum[:, gc], in0=rsum[:, gc], in1=esk[:, gc], op=ALU.add), r=[rsum, esk], w=[rsum])
                        A('dve', lambda e, gc=gc: e.reciprocal(out=rsum[:, gc], in_=rsum[:, gc]), r=[rsum], w=[rsum])
                        tb = PB[5] if g == 0 else PB[7]
                        tv = bfv(tb)
                        for j in range(4):
                            for blk in range(2):
                                A('pe', lambda e, j=j, blk=blk, tv=tv: e.transpose(out=tv[:, (j * 2 + blk) * 128:(j * 2 + blk + 1) * 128], in_=pb[:, j, blk * 128:(blk + 1) * 128], identity=ident[:]), r=[pb, ident], w=[tb])
                        A('act', lambda e, tv=tv: e.copy(out=pT[:].rearrange("p a t -> p (a t)"), in_=tv), r=[tb], w=[pT])
                        for ai in range(2):
                            for w_ in range(2):
                                j = ai * 2 + w_
                                hq = w_ * 4 + g * 2 + ai
                                A('pe', lambda e, j=j, hq=hq, w_=w_: e.matmul(PB[1][:, hq * 64:(hq + 1) * 64], lhsT=pT[:, j * 2, :], rhs=kv_r[prv][:, 128 + w_ * 64:128 + (w_ + 1) * 64], start=True, stop=False), r=[pT, kv_r[prv]], w=[PB[1]])
                                A('pe', lambda e, j=j, hq=hq, w_=w_: e.matmul(PB[1][:, hq * 64:(hq + 1) * 64], lhsT=pT[:, j * 2 + 1, :], rhs=kv_r[cur][:, 128 + w_ * 64:128 + (w_ + 1) * 64], start=False, stop=True), r=[pT, kv_r[cur]], w=[PB[1]])
                        for ai in range(2):
                            for w_ in range(2):
                                j = g * 4 + ai * 2 + w_
                                hq = w_ * 4 + g * 2 + ai
                                A('dve', lambda e, j=j, hq=hq: e.tensor_scalar(out=mix[:, 512 + hq * 64:512 + (hq + 1) * 64], in0=PB[1][:, hq * 64:(hq + 1) * 64], scalar1=rsum[:, j:j + 1], scalar2=None, op0=ALU.mult), r=[PB[1], rsum], w=[mix])
                    pv = bfv(PB[0])
                    for c in range(KC):
                        A('pe', lambda e, c=c: e.transpose(out=pv[:, c * 128:(c + 1) * 128], in_=mix[:, c * 128:(c + 1) * 128], identity=ident[:]), r=[mix, ident], w=[PB[0]])
                    A('act', lambda e: e.copy(out=mixT[:].rearrange("p c t -> p (c t)"), in_=pv), r=[PB[0]], w=[mixT])
                    for dh in range(2):
                        bk = PB[4] if dh == 0 else PB[6]
                        for k in range(KC):
                            A('pe', lambda e, k=k, dh=dh, bk=bk: e.matmul(bk[:], lhsT=mixT[:, k, :], rhs=w_out[:, k, dh * 512:(dh + 1) * 512], start=(k == 0), stop=(k == KC - 1)), r=[mixT, w_out], w=[bk])
                        A('dve', lambda e, dh=dh, bk=bk: e.tensor_tensor(out=tmpm[:, dh * 512:(dh + 1) * 512], in0=bk[:], in1=gt1_b[:, dh * 512:(dh + 1) * 512], op=ALU.mult), r=[bk, gt1_b], w=[tmpm])
                    A('dve', lambda e: e.tensor_tensor(out=x1[:], in0=tmpm[:], in1=xt[:], op=ALU.add), r=[tmpm, xt], w=[x1])
                    A('sp', lambda e: e.dma_start(out=x1s_d[i * 128:(i + 1) * 128, :], in_=x1[:]), r=[x1], dma=True)
                    if dbg:
                        A('sp', lambda e: e.dma_start(out=d_x1[i * 128:(i + 1) * 128, :], in_=x1[:]), r=[x1], dma=True)
                        A('dve', lambda e: e.tensor_copy(out=tmpm[:], in_=mix[:]), r=[mix], w=[tmpm])
                        A('sp', lambda e: e.dma_start(out=d_mix[i * 128:(i + 1) * 128, :], in_=tmpm[:]), r=[tmpm], dma=True)
                    front_from_sbuf(x1, s2c, modcol_sh2, lambda c: h2T[:, c, i * 128:(i + 1) * 128], hT_buf=h2T)
                    for k in range(KC):
                        A('pe', lambda e, k=k: e.matmul(PB[5][:, 0:NEXP], lhsT=h2T[:, k, i * 128:(i + 1) * 128], rhs=w_rt[:, k, :], start=(k == 0), stop=(k == KC - 1)), r=[h2T, w_rt], w=[PB[5]])
                    A('act', lambda e: e.activation(out=sc[:], in_=PB[5][:, 0:NEXP], func=AF.Sigmoid), r=[PB[5]], w=[sc])
                    A('dve', lambda e: e.tensor_tensor(out=sel[:], in0=sc[:], in1=rbias_b[:], op=ALU.add), r=[sc, rbias_b], w=[sel])
                    sel3 = sel[:].rearrange("p (g k) -> p g k", g=8)
                    A('dve', lambda e: e.tensor_reduce(out=m1[:], in_=sel3, axis=AX.X, op=ALU.max), r=[sel], w=[m1])
                    A('dve', lambda e: e.tensor_tensor(out=eqm[:].rearrange("p (g k) -> p g k", g=8), in0=sel3, in1=m1[:].unsqueeze(2).to_broadcast([128, 8, 32]), op=ALU.is_equal), r=[sel, m1], w=[eqm])
                    A('dve', lambda e: e.scalar_tensor_tensor(out=sel2[:], in0=eqm[:], scalar=-1e30, in1=sel[:], op0=ALU.mult, op1=ALU.add), r=[eqm, sel], w=[sel2])
                    A('dve', lambda e: e.tensor_reduce(out=m2[:], in_=sel2[:].rearrange("p (g k) -> p g k", g=8), axis=AX.X, op=ALU.max), r=[sel2], w=[m2])
                    A('dve', lambda e: e.tensor_tensor(out=gs[:], in0=m1[:], in1=m2[:], op=ALU.add), r=[m1, m2], w=[gs])
                    A('dve', lambda e: e.max(out=top8[:], in_=gs[:]), r=[gs], w=[top8])
                    A('dve', lambda e: e.tensor_scalar(out=pen[:], in0=gs[:], scalar1=top8[:, 3:4], scalar2=1e30, op0=ALU.is_lt, op1=ALU.mult), r=[gs, top8], w=[pen])
                    A('dve', lambda e: e.tensor_tensor(out=sel2[:].rearrange("p (g k) -> p g k", g=8), in0=sel3, in1=pen[:].unsqueeze(2).to_broadcast([128, 8, 32]), op=ALU.subtract), r=[sel, pen], w=[sel2])
                    A('dve', lambda e: e.max(out=top8[:], in_=sel2[:]), r=[sel2], w=[top8])
                    A('dve', lambda e: e.tensor_scalar(out=eqm[:], in0=sel2[:], scalar1=top8[:, 7:8], scalar2=None, op0=ALU.is_ge), r=[sel2, top8], w=[eqm])
                    A('dve', lambda e: e.tensor_tensor(out=eqm[:], in0=eqm[:], in1=sc[:], op=ALU.mult), r=[eqm, sc], w=[eqm])
                    A('dve', lambda e: e.tensor_reduce(out=wsum[:], in_=eqm[:], axis=AX.X, op=ALU.add), r=[eqm], w=[wsum])
                    A('dve', lambda e: e.reciprocal(out=wsum[:], in_=wsum[:]), r=[wsum], w=[wsum])
                    A('dve', lambda e: e.tensor_scalar(out=Wr[:, i, 0:NEXP], in0=eqm[:], scalar1=wsum[:, 0:1], scalar2=2.5, op0=ALU.mult, op1=ALU.mult), r=[eqm, wsum], w=[Wr])
                    if dbg:
                        A('sp', lambda e: e.dma_start(out=d_W[i * 128:(i + 1) * 128, :], in_=Wr[:, i, :]), r=[Wr], dma=True)

                for i in range(0 if stop == 'pre' else n_main):
                    main_tile(i)
                S.barrier()
            if stop in ('pre', 'main'):
                raise _Stop()

            with ExitStack() as p2:
                acc = sbuf(p2, "acc", [128, NT, D], F32)
                wgu = [sbuf(p2, "wgu%d" % i, [128, KC, 512], BF16) for i in range(2)]
                wd = [sbuf(p2, "wd%d" % i, [128, 2, D], BF16) for i in range(2)]
                sg = [sbuf(p2, "sg%d" % i, [128, 2, 512], F32) for i in range(2)]
                hm = [sbuf(p2, "hm%d" % i, [128, 2, 512], BF16) for i in range(2)]
                xf = sbuf(p2, "xf", [128, D], F32)
                xl = sbuf(p2, "xl", [128, D], F32)
                sq2 = sbuf(p2, "sq2", [128, D], F32)
                ss2 = sbuf(p2, "ss2", [128, 4], F32)
                ot = sbuf(p2, "ot", [128, D], F32)
                A('pool', lambda e: e.memset(acc[:], 0.0), w=[acc])
                accr = [Res("acc%d" % t) for t in range(NT)]
                for t in range(NT):
                    accr[t].w = acc.r.w

                wst = [sbuf(p2, "wst%d" % i, [128, 2048], F32) for i in range(2)]
                ldc = [0]

                def load_w(ei):
                    s_ = ei % 2
                    if ei < NEXP:
                        g_src, u_src, d_src = w_gate_d[ei], w_up_d[ei], w_down_d[ei]
                    else:
                        g_src, u_src, d_src = ws_gate_d, ws_up_d, ws_down_d
                    for which, src in enumerate((g_src, u_src, d_src)):
                        st = wst[ldc[0] % 2]
                        ldc[0] += 1
                        if which < 2:
                            A('sp', lambda e, st=st, src=src: e.dma_start(out=st[:].rearrange("p (c f) -> p c f", c=KC), in_=src.rearrange("(c p) f -> p c f", p=128)), w=[st], dma=True)
                            A('pool', lambda e, st=st, which=which, s_=s_: e.tensor_copy(out=wgu[s_][:, :, which * 256:(which + 1) * 256], in_=st[:].rearrange("p (c f) -> p c f", c=KC)), r=[st], w=[wgu[s_]])
                        else:
                            A('sp', lambda e, st=st, src=src: e.dma_start(out=st[:].rearrange("p (c f) -> p c f", c=2), in_=src.rearrange("(c p) f -> p c f", p=128)), w=[st], dma=True)
                            A('pool', lambda e, st=st, s_=s_: e.tensor_copy(out=wd[s_][:].rearrange("p c f -> p (c f)"), in_=st[:]), r=[st], w=[wd[s_]])

                elist = list(range(n_exp_decl)) + [NEXP]
                steps = [(idx, ei, tg) for idx, ei in enumerate(elist) for tg in range(4)]

                def rec_y_tile(n, t4):
                    _, ei, tg = steps[n]
                    s = ei % 2
                    par = n % 2
                    tile = tg * 4 + t4
                    yb = (PB[4], PB[5]) if t4 % 2 == 0 else (PB[6], PB[7])
                    for dh in range(2):
                        for half in range(2):
                            A('pe', lambda e, dh=dh, half=half: e.matmul(yb[dh][:], lhsT=hm[par][:, half, t4 * 128:(t4 + 1) * 128], rhs=wd[s][:, half, dh * 512:(dh + 1) * 512], start=(half == 0), stop=(half == 1)), r=[hm[par], wd[s]], w=[yb[dh]])
                    for dh in range(2):
                        S.op('dve', lambda e, dh=dh: e.scalar_tensor_tensor(out=acc[:, tile, dh * 512:(dh + 1) * 512], in0=yb[dh][:], scalar=Wr[:, tile, ei:ei + 1], in1=acc[:, tile, dh * 512:(dh + 1) * 512], op0=ALU.mult, op1=ALU.add), reads=[yb[dh].r, Wr.r, accr[tile]], writes=[accr[tile]])

                load_w(elist[0])
                for n, (idx, ei, tg) in enumerate(steps):
                    s = ei % 2
                    par = n % 2
                    tsl = slice(tg * 512, (tg + 1) * 512)
                    for gi, (kind, half) in enumerate((('G', 0), ('U', 0), ('G', 1), ('U', 1))):
                        if n > 0:
                            rec_y_tile(n - 1, gi)
                        if kind == 'G':
                            for k in range(KC):
                                A('pe', lambda e, k=k, half=half, s=s, tsl=tsl: e.matmul(PB[half][:], lhsT=wgu[s][:, k, half * 128:(half + 1) * 128], rhs=h2T[:, k, tsl], start=(k == 0), stop=(k == KC - 1)), r=[wgu[s], h2T], w=[PB[half]])
                            A('act', lambda e, half=half, par=par: e.activation(out=sg[par][:, half, :], in_=PB[half][:], func=AF.Silu), r=[PB[half]], w=[sg[par]])
                        else:
                            for k in range(KC):
                                A('pe', lambda e, k=k, half=half, s=s, tsl=tsl: e.matmul(PB[2 + half][:], lhsT=wgu[s][:, k, 256 + half * 128:256 + (half + 1) * 128], rhs=h2T[:, k, tsl], start=(k == 0), stop=(k == KC - 1)), r=[wgu[s], h2T], w=[PB[2 + half]])
                            A('dve', lambda e, half=half, par=par: e.scalar_tensor_tensor(out=hm[par][:, half, :], in0=PB[2 + half][:], scalar=1.0, in1=sg[par][:, half, :], op0=ALU.mult, op1=ALU.mult), r=[PB[2 + half], sg[par]], w=[hm[par]])
                    if tg == 0 and idx + 1 < len(elist):
                        load_w(elist[idx + 1])
                for gi in range(4):
                    rec_y_tile(len(steps) - 1, gi)
                for t in range(NT):
                    A('sp', lambda e, t=t: e.dma_start(out=xl[:], in_=x1s_d[t * 128:(t + 1) * 128, :]), w=[xl], dma=True)
                    S.op('dve', lambda e, t=t: e.tensor_tensor(out=xf[:], in0=acc[:, t, :], in1=gt2_b[:], op=ALU.mult), reads=[accr[t], gt2_b.r], writes=[xf.r])
                    A('dve', lambda e: e.tensor_tensor(out=xf[:], in0=xf[:], in1=xl[:], op=ALU.add), r=[xf, xl], w=[xf])
                    A('act', lambda e: e.activation(out=sq2[:], in_=xf[:], func=AF.Square, accum_out=ss2[:, 0:1]), r=[xf], w=[sq2, ss2])
                    A('act', lambda e: e.activation(out=ss2[:, 1:2], in_=ss2[:, 0:1], func=AF.Sqrt, scale=1.0 / D, bias=EPS), r=[ss2], w=[ss2])
                    A('dve', lambda e: e.reciprocal(out=ss2[:, 2:3], in_=ss2[:, 1:2]), r=[ss2], w=[ss2])
                    A('dve', lambda e: e.scalar_tensor_tensor(out=ot[:], in0=xf[:], scalar=ss2[:, 2:3], in1=gfin_b[:], op0=ALU.mult, op1=ALU.mult), r=[xf, ss2, gfin_b], w=[ot])
                    A('sp', lambda e, t=t: e.dma_start(out=out_d[t * 128:(t + 1) * 128, :], in_=ot[:]), r=[ot], dma=True)
        except _Stop:
            dummy = sbuf(es, 'dummy', [128, D], F32)
            A('dve', lambda e: e.memset(dummy[:], 1.0), w=[dummy])
            A('sp', lambda e: e.dma_start(out=out_d[0:128, :], in_=dummy[:]), r=[dummy], dma=True)
        S.wait_all('sp')
        stats = S.emit()
    return nc, stats


_CACHE = {}


def make_in_maps(x, c, positions, w_ada, b_ada, g_attn, w_in, w_gk_up, b_gk, g_gla_out, sinks, w_out,
                 g_ffn, w_router, router_bias, w_gate, w_up, w_down, ws_gate, ws_up, ws_down, g_final):
    f = np.float32
    x = np.asarray(x, f)
    shared = {
        "w_ada": np.ascontiguousarray(np.asarray(w_ada, f)[0]),
        "b_ada_col": np.ascontiguousarray(np.asarray(b_ada, f)[0].reshape(48, 128).T),
        "b_ada": np.ascontiguousarray(np.asarray(b_ada, f)[0].reshape(1, -1)),
        "g_attn_col": np.ascontiguousarray(np.asarray(g_attn, f)[0].reshape(8, 128).T),
        "g_ffn_col": np.ascontiguousarray(np.asarray(g_ffn, f)[0].reshape(8, 128).T),
        "g_final": np.ascontiguousarray(np.asarray(g_final, f).reshape(1, -1)),
        "w_in": np.ascontiguousarray(np.asarray(w_in, f)[0]),
        "w_gk_up": np.ascontiguousarray(np.asarray(w_gk_up, f)[0]),
        "b_gk": np.ascontiguousarray(np.asarray(b_gk, f)[0].reshape(1, -1)),
        "g_gla": np.ascontiguousarray(np.asarray(g_gla_out, f)[0].reshape(1, -1)),
        "sinks": np.ascontiguousarray(np.asarray(sinks, f)[0].reshape(1, -1)),
        "w_out": np.ascontiguousarray(np.asarray(w_out, f)[0]),
        "w_router": np.ascontiguousarray(np.asarray(w_router, f)[0]),
        "router_bias": np.ascontiguousarray(np.asarray(router_bias, f)[0].reshape(1, -1)),
        "w_gate": np.ascontiguousarray(np.asarray(w_gate, f)[0]),
        "w_up": np.ascontiguousarray(np.asarray(w_up, f)[0]),
        "w_down": np.ascontiguousarray(np.asarray(w_down, f)[0]),
        "ws_gate": np.ascontiguousarray(np.asarray(ws_gate, f)[0]),
        "ws_up": np.ascontiguousarray(np.asarray(ws_up, f)[0]),
        "ws_down": np.ascontiguousarray(np.asarray(ws_down, f)[0]),
    }
    positions = np.asarray(positions, np.int32)
    c = np.asarray(c, f)
    qi = np.arange(128)[:, None]
    kj = np.arange(128)[None, :]
    band = np.concatenate([np.where(kj > qi, 0.0, NEG), np.where(kj <= qi, 0.0, NEG)], axis=1).astype(f)
    in_maps = []
    for core in range(N_CORES):
        b, s = core // 4, core % 4
        t0 = s * TOK
        m = dict(shared)
        m["x_main"] = np.ascontiguousarray(x[b, t0:t0 + TOK])
        xp = np.zeros((NPRE * T, D), f)
        va = np.zeros((NPRE * T,), f)
        if s > 0:
            xp[NPRE * T - t0:] = x[b, :t0]
            va[NPRE * T - t0:] = 1.0
        m["x_pre"] = xp
        m["valid"] = np.ascontiguousarray(va.reshape(NPRE, T).T)
        pos = np.zeros((T, NT + 1), np.int32)
        pos[:, 1:] = positions[b, t0:t0 + TOK].reshape(NT, T).T
        if s > 0:
            pos[:, 0] = positions[b, t0 - T:t0]
        m["pos"] = pos
        mk = band.copy()
        if s == 0:
            mk[:, 0:128] = NEG
        m["mask0"] = mk
        m["c_col"] = np.ascontiguousarray(c[b].reshape(8, 128).T)
        in_maps.append(m)
    return in_maps


def kernel(**inputs):
    if "nc" not in _CACHE:
        _CACHE["nc"] = build_program()[0]
    nc = _CACHE["nc"]
    in_maps = make_in_maps(**inputs)
    res = run_bass_kernel_spmd(nc, in_maps, core_ids=list(range(N_CORES)))
    out = np.zeros((2, 4 * TOK, D), np.float32)
    for core in range(N_CORES):
        b, s = core // 4, core % 4
        out[b, s * TOK:(s + 1) * TOK] = res.results[core]["out"]
    return out
```
